# Optimizing a Trainium2 kernel written in Bass

```python
import jax, jax.numpy as jnp
from jax import lax
import numpy as np

D_MODEL = 1024
BATCH = 32
SEQ = 2048
DEPTH = 1

A_GROUPS = 8
A_GROUP_DIM = 64
A_WIDTH = A_GROUPS * A_GROUP_DIM
CHUNK = 128
N_HEADS = 8
N_KV_HEADS = 2
HEAD_DIM = 64
Q_GROUP = N_HEADS // N_KV_HEADS
B_WIDTH = N_HEADS * HEAD_DIM
KV_WIDTH = N_KV_HEADS * HEAD_DIM
WINDOW = 128
BLOCK = 128
SPAN = BLOCK + 2 * WINDOW
N_BUCKETS = 32
MAX_DISTANCE = 128
N_EXPERTS = 16
EXPERT_FF = 2048
CAPACITY_FACTOR = 2
EPS = 1e-6
IN_COLS = 2 * A_WIDTH + B_WIDTH + 2 * KV_WIDTH + 2 * D_MODEL
SPLITS = (A_WIDTH, 2 * A_WIDTH, 2 * A_WIDTH + B_WIDTH, 2 * A_WIDTH + B_WIDTH + KV_WIDTH,
          2 * A_WIDTH + B_WIDTH + 2 * KV_WIDTH, 2 * A_WIDTH + B_WIDTH + 2 * KV_WIDTH + D_MODEL)

kernel_name = "hybrid_gmlp_swa_ec_moe_encoder"


def rms_norm(x, g):
    xf = x.astype(jnp.float32)
    y = xf * lax.rsqrt(jnp.mean(xf * xf, axis=-1, keepdims=True) + EPS)
    return (y * g.astype(jnp.float32)).astype(x.dtype)


def t5_bucket(rel):
    nb = N_BUCKETS // 2
    max_exact = nb // 2
    ret = (rel > 0).astype(np.int32) * nb
    n = np.abs(rel)
    large = max_exact + (np.log(np.maximum(n, 1) / max_exact) / np.log(MAX_DISTANCE / max_exact)
                         * (nb - max_exact)).astype(np.int32)
    large = np.minimum(large, nb - 1)
    return (ret + np.where(n < max_exact, n, large)).astype(np.int32)


def chunked_spatial_gating(u, v, vnorm_g, w_spatial, b_spatial):
    B, S, _ = v.shape
    v = rms_norm(v, vnorm_g)
    vc = v.reshape(B, S // CHUNK, CHUNK, A_GROUPS, A_GROUP_DIM)
    mixed = jnp.einsum('gpq,bcqgd->bcpgd', w_spatial, vc) + b_spatial.T[None, None, :, :, None]
    return u * mixed.reshape(B, S, A_WIDTH)


def windowed_gqa(q, k, v, sink, rel_table):
    B, S = q.shape[0], q.shape[1]
    nb = S // BLOCK
    qb = q.reshape(B, nb, BLOCK, N_KV_HEADS, Q_GROUP, HEAD_DIM).transpose(1, 0, 3, 4, 2, 5)
    pad = ((0, 0), (WINDOW, WINDOW), (0, 0), (0, 0))
    kp = jnp.pad(k, pad).transpose(0, 2, 1, 3).astype(jnp.float32)
    vp = jnp.pad(v, pad).transpose(0, 2, 1, 3).astype(jnp.float32)
    rel = (np.arange(SPAN)[None, :] - WINDOW) - np.arange(BLOCK)[:, None]
    band = jnp.asarray(np.abs(rel) <= WINDOW)
    bias = rel_table[t5_bucket(rel)].astype(jnp.float32)
    bias = bias.transpose(2, 0, 1).reshape(N_KV_HEADS, Q_GROUP, BLOCK, SPAN)
    sink_f = sink.astype(jnp.float32).reshape(N_KV_HEADS, Q_GROUP, 1, 1)
    scale = HEAD_DIM ** -0.5

    def one_block(args):
        n, qblk = args
        start = n * BLOCK
        kblk = lax.dynamic_slice_in_dim(kp, start, SPAN, axis=2)
        vblk = lax.dynamic_slice_in_dim(vp, start, SPAN, axis=2)
        pos = start - WINDOW + jnp.arange(SPAN)
        valid = band & ((pos >= 0) & (pos < S))[None, :]
        s = jnp.einsum('bkgqd,bkjd->bkgqj', qblk.astype(jnp.float32), kblk) * scale + bias
        s = jnp.where(valid, s, -jnp.inf)
        m = jnp.maximum(jnp.max(s, axis=-1, keepdims=True), sink_f)
        p = jnp.exp(s - m)
        denom = jnp.sum(p, axis=-1, keepdims=True) + jnp.exp(sink_f - m)
        o = jnp.einsum('bkgqj,bkjd->bkgqd', p, vblk) / denom
        return o.astype(q.dtype)

    out = lax.map(one_block, (jnp.arange(nb), qb))
    return out.transpose(1, 0, 4, 2, 3, 5).reshape(B, S, B_WIDTH)


def expert_choice_moe(h, w_router, w_gate_e, w_up_e, w_down_e):
    B, S, _ = h.shape
    cap = CAPACITY_FACTOR * S // N_EXPERTS
    logits = jnp.einsum('bsd,de->bse', h.astype(jnp.float32), w_router.astype(jnp.float32))
    aff = jax.nn.softmax(logits, axis=-1)
    gates, idx = lax.top_k(aff.transpose(0, 2, 1), cap)
    bidx = jnp.arange(B)[:, None, None]
    xg = h[bidx, idx]
    hid = jax.nn.silu(jnp.einsum('becd,edf->becf', xg, w_gate_e)) * jnp.einsum('becd,edf->becf', xg, w_up_e)
    out = jnp.einsum('becf,efd->becd', hid, w_down_e) * gates[..., None].astype(h.dtype)
    return jnp.zeros_like(h).at[bidx, idx].add(out)


def setup_inputs(seed: int = 0) -> dict:
    key = jax.random.key(seed)
    ks = jax.random.split(key, 20)
    f32 = jnp.float32
    L = DEPTH
    nrm = lambda k, shape, s: jax.random.normal(k, shape, f32) * s
    return {
        "x": jax.random.normal(ks[0], (BATCH, SEQ, D_MODEL), f32),
        "norm_mix_g": 1.0 + nrm(ks[1], (L, D_MODEL), 0.02),
        "w_in": nrm(ks[2], (L, D_MODEL, IN_COLS), D_MODEL ** -0.5),
        "b_gate": nrm(ks[3], (L, 2 * D_MODEL), 0.02),
        "vnorm_g": 1.0 + nrm(ks[4], (L, A_WIDTH), 0.02),
        "w_spatial": nrm(ks[5], (L, A_GROUPS, CHUNK, CHUNK), CHUNK ** -0.5),
        "b_spatial": 1.0 + nrm(ks[6], (L, A_GROUPS, CHUNK), 0.02),
        "q_norm_g": 1.0 + nrm(ks[7], (L, HEAD_DIM), 0.02),
        "k_norm_g": 1.0 + nrm(ks[8], (L, HEAD_DIM), 0.02),
        "attn_sink": nrm(ks[9], (L, N_HEADS), 1.0),
        "rel_bias_table": nrm(ks[10], (N_BUCKETS, N_HEADS), 0.5),
        "w_proj_a": nrm(ks[11], (L, A_WIDTH, D_MODEL), A_WIDTH ** -0.5),
        "w_proj_b": nrm(ks[12], (L, B_WIDTH, D_MODEL), B_WIDTH ** -0.5),
        "w_out": nrm(ks[13], (L, D_MODEL, D_MODEL), D_MODEL ** -0.5),
        "norm_ffn_g": 1.0 + nrm(ks[14], (L, D_MODEL), 0.02),
        "w_router": nrm(ks[15], (L, D_MODEL, N_EXPERTS), D_MODEL ** -0.5),
        "w_gate_e": nrm(ks[16], (L, N_EXPERTS, D_MODEL, EXPERT_FF), D_MODEL ** -0.5),
        "w_up_e": nrm(ks[17], (L, N_EXPERTS, D_MODEL, EXPERT_FF), D_MODEL ** -0.5),
        "w_down_e": nrm(ks[18], (L, N_EXPERTS, EXPERT_FF, D_MODEL), EXPERT_FF ** -0.5),
    }


def reference(x, norm_mix_g, w_in, b_gate, vnorm_g, w_spatial, b_spatial, q_norm_g, k_norm_g,
              attn_sink, rel_bias_table, w_proj_a, w_proj_b, w_out, norm_ffn_g, w_router,
              w_gate_e, w_up_e, w_down_e):
    B, S, _ = x.shape
    for l in range(DEPTH):
        h = rms_norm(x, norm_mix_g[l])
        z = jnp.einsum('bsd,dc->bsc', h, w_in[l])
        u_a, v_a, q, k, v, g_a, g_b = jnp.split(z, SPLITS, axis=-1)
        a = chunked_spatial_gating(jax.nn.gelu(u_a), jax.nn.gelu(v_a), vnorm_g[l], w_spatial[l], b_spatial[l])
        q = rms_norm(q.reshape(B, S, N_HEADS, HEAD_DIM), q_norm_g[l])
        k = rms_norm(k.reshape(B, S, N_KV_HEADS, HEAD_DIM), k_norm_g[l])
        v = v.reshape(B, S, N_KV_HEADS, HEAD_DIM)
        o = windowed_gqa(q, k, v, attn_sink[l], rel_bias_table)
        gate_a = jax.nn.sigmoid(g_a + b_gate[l, :D_MODEL])
        gate_b = jax.nn.sigmoid(g_b + b_gate[l, D_MODEL:])
        merged = gate_a * jnp.einsum('bsa,ad->bsd', a, w_proj_a[l]) + gate_b * jnp.einsum('bsa,ad->bsd', o, w_proj_b[l])
        x = x + jnp.einsum('bsd,de->bse', merged, w_out[l])
        h = rms_norm(x, norm_ffn_g[l])
        x = x + expert_choice_moe(h, w_router[l], w_gate_e[l], w_up_e[l], w_down_e[l])
    return x
```

```python
import contextlib
import numpy as np
import concourse.bass as bass
import concourse.mybir as mybir
from concourse.bass_utils import run_bass_kernel_spmd

F32, BF16, U32 = mybir.dt.float32, mybir.dt.bfloat16, mybir.dt.uint32
AF = mybir.ActivationFunctionType
ALU = mybir.AluOpType
AX = mybir.AxisListType

D = 1024
S = 2048
NB = S // 128
E = 16
FF = 2048
CAP = 256
IN_COLS = 3840
EPS = 1e-6
NCORES = 8
CUT = 99


class Buf:
    __slots__ = ("name", "w", "r", "wm")

    def __init__(self, name=""):
        self.name = name
        self.w = None
        self.r = {}
        self.wm = {}


class Dep:
    def __init__(self, nc, es, ndma=8):
        self.nc = nc
        self.E = dict(pe=nc.tensor, dve=nc.vector, act=nc.scalar, pool=nc.gpsimd, sp=nc.sync)
        self.csem = {k: es.enter_context(nc.semaphore("c_" + k)) for k in ("pe", "dve", "act", "pool")}
        self.ccnt = {k: 0 for k in self.csem}
        self.seen = {k: {} for k in self.E}
        self.pend = {k: [] for k in self.csem}
        self.dsem = {q: [es.enter_context(nc.semaphore(f"d_{q}{i}")) for i in range(ndma)] for q in ("sp", "pool")}
        self.dval = {q: [0] * ndma for q in self.dsem}
        self.dpos = {q: 0 for q in self.dsem}
        self.ndma = ndma

    def wait(self, ek, ev):
        key, sem, val = ev
        if ek == "pe" and key == "c_pe":
            return
        if self.seen[ek].get(key, 0) >= val:
            return
        self.E[ek].wait_ge(sem, val)
        self.seen[ek][key] = val

    def _deps(self, ek, reads, writes, mwrites=()):
        for b in reads:
            assert not any(pb is b and md == "w" for pe_ in self.pend.values() for (pb, md) in pe_), \
                f"read of unsignaled buffer {b.name}"
            if b.w is not None:
                self.wait(ek, b.w)
            for ev in b.wm.values():
                self.wait(ek, ev)
        for b in mwrites:
            if b.w is not None:
                self.wait(ek, b.w)
            for ev in b.r.values():
                self.wait(ek, ev)
        for b in writes:
            for ev in b.wm.values():
                self.wait(ek, ev)
            for k2, pl in self.pend.items():
                if k2 == ek == "pe":
                    continue
                assert not any(pb is b for (pb, md) in pl), f"write to buffer with unsignaled access {b.name}"
            if b.w is not None:
                self.wait(ek, b.w)
            for ev in b.r.values():
                self.wait(ek, ev)

    @staticmethod
    def _reg(ev, reads, writes):
        for b in reads:
            old = b.r.get(ev[0])
            if old is None or old[2] < ev[2]:
                b.r[ev[0]] = ev
        for b in writes:
            b.w = ev
            b.r = {}
            b.wm = {}

    def op(self, ek, fn, reads=(), writes=(), signal=True):
        self._deps(ek, reads, writes)
        inst = fn(self.E[ek])
        if signal:
            self.ccnt[ek] += 1
            inst.then_inc(self.csem[ek], 1)
            ev = ("c_" + ek, self.csem[ek], self.ccnt[ek])
            for (pb, md) in self.pend[ek]:
                self._reg(ev, [pb] if md == "r" else [], [pb] if md == "w" else [])
            self.pend[ek] = []
            self._reg(ev, reads, writes)
            return ev
        for b in reads:
            self.pend[ek].append((b, "r"))
        for b in writes:
            self.pend[ek].append((b, "w"))
        return None

    def dma(self, q, fn, reads=(), writes=(), mwrites=()):
        self._deps(q, reads, writes, mwrites)
        j = self.dpos[q]
        self.dpos[q] = (j + 1) % self.ndma
        sem = self.dsem[q][j]
        key = f"d_{q}{j}"
        if self.dval[q][j] > 0:
            self.wait(q, (key, sem, self.dval[q][j]))
        inst = fn(self.E[q])
        self.dval[q][j] += 16
        inst.then_inc(sem, 16)
        ev = (key, sem, self.dval[q][j])
        self._reg(ev, reads, writes)
        for b in mwrites:
            old = b.wm.get(key)
            if old is None or old[2] < ev[2]:
                b.wm[key] = ev
        return ev


def _t5_bucket(rel):
    nb = 16
    max_exact = 8
    ret = (rel > 0).astype(np.int32) * nb
    n = np.abs(rel)
    large = max_exact + (np.log(np.maximum(n, 1) / max_exact) / np.log(128 / max_exact)
                         * (nb - max_exact)).astype(np.int32)
    large = np.minimum(large, nb - 1)
    return (ret + np.where(n < max_exact, n, large)).astype(np.int32)


QPERM = [0, 4, 1, 5, 2, 6, 3, 7]


def build(NSEQ=4, upto="C", dbg=False):
    nc = bass.Bass("TRN2", target_bir_lowering=False)
    NT = NSEQ * S
    NBLK = NSEQ * NB

    def din(name, shape, dt=F32):
        return nc.dram_tensor(name, list(shape), dt, kind="ExternalInput").ap()

    x_d = din("x", [NT, D])
    g1_d = din("norm_mix_g", [D])
    win_d = din("w_in", [D, IN_COLS])
    bg_d = din("b_gate", [2 * D])
    vg_d = din("vnorm_g", [512])
    wsp_d = din("w_spatial", [8, 128, 128])
    bsp_d = din("b_spatial", [8, 128])
    qg_d = din("q_norm_g", [64])
    kg_d = din("k_norm_g", [64])
    sink_d = din("attn_sink", [8])
    bias_d = din("bias_exp", [128, 3 * 8 * 128])
    mask_d = din("band_mask", [128, 3 * 8 * 128])
    wpa_d = din("w_proj_a", [512, D])
    wpb_d = din("w_proj_b", [512, D])
    wout_d = din("w_out", [D, D])
    g2_d = din("norm_ffn_g", [D])
    wr_d = din("w_router", [D, E])
    EDECL = E if upto == "C" else 1
    wge_d = din("w_gate_e", [EDECL, D, FF])
    wue_d = din("w_up_e", [EDECL, D, FF])
    wde_d = din("w_down_e", [EDECL, FF, D])
    ident_d = din("ident", [128, 128])
    offs_d = din("offs", [128, 1])
    flip_d = din("flip", [128, 128])

    out_d = nc.dram_tensor("out", [NT, D], F32, kind="ExternalOutput").ap()
    h2_d = nc.dram_tensor("h2d", [NT, D], BF16, kind="ExternalOutput" if dbg else "Internal").ap()
    if dbg:
        aff_dbg = nc.dram_tensor("aff_dbg", [128, S // 2], F32, kind="ExternalOutput").ap()
        idx_dbg = nc.dram_tensor("idx_dbg", [128, 2 * 64], U32, kind="ExternalOutput").ap()
        gat_dbg = nc.dram_tensor("gat_dbg", [128, 2 * 64], F32, kind="ExternalOutput").ap()

    es = contextlib.ExitStack()
    with es:
        dp = Dep(nc, es)
        op, dma = dp.op, dp.dma

        def sb(name, shape, dt):
            return es.enter_context(nc.sbuf_tensor(name, list(shape), dt))

        ps = es.enter_context(nc.psum_tensor("ps", [128, 8, 512], F32))
        psb = [Buf(f"psum{i}") for i in range(8)]
        ring = [0]

        def palloc(n=1):
            p = ring[0]
            if n == 2 and p % 2 == 1:
                p = (p + 1) % 8
            ring[0] = (p + n) % 8
            return p

        def pbf(b):
            return ps[:, b, :].bitcast(BF16)

        ident_f = sb("ident_f", [128, 128], F32)
        ident_b = sb("ident_b", [128, 128], BF16)
        neghalf = sb("neghalf", [128, 16], F32)
        aff_all = sb("aff_all", [128, S // 2], F32)
        flipm = sb("flipm", [128, 128], F32)
        B_ident = Buf("ident")
        B_aff_all = Buf("aff_all")
        B_const = Buf("const")

        dma("sp", lambda e: e.dma_start(out=ident_f[:], in_=ident_d[:, :]), writes=[B_ident])
        dma("sp", lambda e: e.dma_start(out=flipm[:], in_=flip_d[:, :]), writes=[B_ident])
        op("pool", lambda e: e.memset(aff_all[:], 0.0), writes=[B_aff_all])
        dma("pool", lambda e: e.dma_start(out=ident_b[:], in_=ident_d[:, :]), writes=[B_ident])
        op("pool", lambda e: e.memset(neghalf[:], -0.5), writes=[B_const])

        B_out = [Buf(f"out{s}") for s in range(NSEQ)]
        B_h2d = [Buf(f"h2d{s}") for s in range(NSEQ)]

        def rstd_from_ss(ss_ap, rstd_ap, n, inv_n, rb, wb, tmp_ap, tb):
            op("pool", lambda e: e.tensor_scalar(out=tmp_ap, in0=ss_ap, scalar1=inv_n, scalar2=EPS,
                                                 op0=ALU.mult, op1=ALU.add), reads=[rb], writes=[tb])
            op("pool", lambda e: e.tensor_tensor(out=rstd_ap, in0=tmp_ap, in1=neghalf[:, 0:n], op=ALU.pow),
               reads=[tb, B_const], writes=[wb])

        with contextlib.ExitStack() as esA:
            def sba(name, shape, dt):
                return esA.enter_context(nc.sbuf_tensor(name, list(shape), dt))

            W_in = sba("W_in", [128, 8, IN_COLS], BF16)
            Wpa = sba("Wpa", [128, 4, D], BF16)
            Wpb = sba("Wpb", [128, 4, D], BF16)
            Wout = sba("Wout", [128, 8, D], BF16)
            WsN = sba("WsN", [128, 8, 128], BF16)
            WsT = sba("WsT", [128, 8, 128], BF16)
            Wr = sba("Wr", [128, 8, E], F32)
            g1B = sba("g1B", [128, D], F32)
            g2B = sba("g2B", [128, D], F32)
            vgB = sba("vgB", [128, 512], F32)
            gqk = sba("gqk", [128, 128], F32)
            gkB = sba("gkB", [128, 64], F32)
            g2c = sba("g2c", [128, 8], F32)
            bsp = sba("bsp", [128, 8], F32)
            esink = sba("esink", [128, 8], F32)
            expb = sba("expb", [128, 3 * 8 * 128], BF16)
            B_W = Buf("paramsA")
            B_bg = Buf("bgrow")
            B_Wing = [Buf(f"W_in_g{i}") for i in range(8)]
            B_Win = [Buf(f"W_in{i}") for i in range(8)]
            B_Wp = [Buf(f"Wp{i}") for i in range(8)]
            B_Wo = [Buf(f"Wout{i}") for i in range(8)]
            B_Ws, B_Wr, B_eb, B_bsp = Buf("Ws"), Buf("Wr"), Buf("expb"), Buf("bsp")
            bgrow = sba("bgrow", [128, 2 * D], BF16)
            onesr = sba("onesr", [128, 128], BF16)

            dma("pool", lambda e: e.dma_start(out=WsN[:], in_=wsp_d.rearrange("g p q -> p g q")), writes=[B_Ws])
            with nc.allow_non_contiguous_dma(reason="tiny one-time parameter loads"):
                dma("pool", lambda e: e.dma_start(out=bsp[:], in_=bsp_d.rearrange("g p -> p g")), writes=[B_bsp])
            for (c0, c1, bw) in ((0, 1792, B_Win), (1792, IN_COLS, B_Wing)):
                for kc in range(8):
                    dma("pool", lambda e, kc=kc, c0=c0, c1=c1: e.dma_start(
                        out=W_in[:, kc, c0:c1], in_=win_d[kc * 128:(kc + 1) * 128, c0:c1]), writes=[bw[kc]])
            dma("sp", lambda e: e.dma_start(out=g1B[:], in_=g1_d.partition_broadcast(128)), writes=[B_W])
            dma("sp", lambda e: e.dma_start(out=vgB[:], in_=vg_d.partition_broadcast(128)), writes=[B_W])
            dma("sp", lambda e: e.dma_start(out=gqk[:, 0:64], in_=qg_d.partition_broadcast(128)), writes=[B_W])
            dma("sp", lambda e: e.dma_start(out=gkB[:], in_=kg_d.partition_broadcast(128)), writes=[B_W])
            dma("sp", lambda e: e.dma_start(out=esink[:], in_=sink_d.partition_broadcast(128)), writes=[B_W])
            dma("sp", lambda e: e.dma_start(out=g2B[:], in_=g2_d.partition_broadcast(128)), writes=[B_W])
            op("pool", lambda e: e.memset(bgrow[:], 0.0), writes=[B_bg])
            dma("pool", lambda e: e.dma_start(out=bgrow[0:1, :], in_=bg_d.unsqueeze(0)), writes=[B_bg])
            for kc in range(4):
                dma("pool", lambda e, kc=kc: e.dma_start(out=Wpa[:, kc, :], in_=wpa_d[kc * 128:(kc + 1) * 128, :]), writes=[B_Wp[kc]])
                dma("pool", lambda e, kc=kc: e.dma_start(out=Wpb[:, kc, :], in_=wpb_d[kc * 128:(kc + 1) * 128, :]), writes=[B_Wp[4 + kc]])
            for kc in range(8):
                dma("pool", lambda e, kc=kc: e.dma_start(out=Wout[:, kc, :], in_=wout_d[kc * 128:(kc + 1) * 128, :]), writes=[B_Wo[kc]])
            dma("pool", lambda e: e.dma_start(out=Wr[:], in_=wr_d.rearrange("(k p) e -> p k e", p=128)), writes=[B_Wr])
            with nc.allow_non_contiguous_dma(reason="tiny one-time parameter loads"):
                dma("pool", lambda e: e.dma_start(out=g2c[:], in_=g2_d.rearrange("(k p) -> p k", p=128)), writes=[B_Wr])
            op("dve", lambda e: e.memset(onesr[:], 1.0), writes=[B_W])

            op("dve", lambda e: e.scalar_tensor_tensor(out=gqk[:, 0:64], in0=gqk[:, 0:64], scalar=0.125, in1=gkB[:],
                                                       op0=ALU.mult, op1=ALU.mult), reads=[B_W], writes=[B_W])
            op("dve", lambda e: e.tensor_copy(out=gqk[:, 64:128], in_=gqk[:, 0:64]), reads=[B_W], writes=[B_W])
            op("act", lambda e: e.activation(out=esink[:], in_=esink[:], func=AF.Exp), reads=[B_W], writes=[B_W])
            t1 = sba("t1", [128, D], F32)
            B_t1 = Buf("t1")
            t2 = sba("t2", [128, D], F32)
            B_t2 = [Buf("t2a"), Buf("t2b")]
            pb_ = palloc()
            for g in range(8):
                op("pe", lambda e, g=g: e.transpose(out=pbf(pb_)[:, g * 128:(g + 1) * 128], in_=WsN[:, g, :],
                                                    identity=ident_b[:]),
                   reads=[B_Ws, B_ident], writes=[psb[pb_]], signal=(g == 7))
            op("act", lambda e: e.copy(out=WsT[:].rearrange("p g q -> p (g q)"), in_=pbf(pb_)[:, :]),
               reads=[psb[pb_]], writes=[B_Ws])

            xt = [sba(f"xt{i}", [128, D], F32) for i in range(4)]
            B_xt = [Buf(f"xt{i}") for i in range(4)]
            junk = [sba(f"junk{i}", [128, D if i != 1 else 512], BF16) for i in range(3)]
            B_junk = [Buf(f"junk{i}") for i in range(3)]
            st_ = sba("stats", [128, 64], F32)
            hb = [sba(f"hb{i}", [128, D], BF16) for i in range(2)]
            B_hb = [Buf(f"hb{i}") for i in range(2)]
            hT = [sba(f"hT{i}", [128, 8, 128], BF16) for i in range(2)]
            B_hT = [Buf(f"hT{i}") for i in range(2)]
            u = sba("u", [128, 512], BF16)
            B_u = Buf("u")
            va = sba("va", [128, 512], F32)
            B_va = Buf("va")
            vn = sba("vn", [128, 512], BF16)
            B_vn = Buf("vn")
            a_ = sba("a", [128, 512], BF16)
            B_a = [Buf(f"a{g}") for g in range(8)]
            aT = [sba(f"aT{i}", [128, 4, 128], BF16) for i in range(2)]
            B_aT = [Buf(f"aT{i}") for i in range(2)]
            sq = sba("sq", [128, 640], F32)
            B_sq = Buf("sq")
            qn = sba("qn", [128, 640], BF16)
            B_qn = Buf("qn")
            knt = sba("knt", [128, 128], F32)
            B_knt = Buf("knt")
            qT = [sba(f"qT{i}", [128, 4, 128], BF16) for i in range(2)]
            B_qT = [Buf(f"qT{i}") for i in range(2)]
            kT = sba("kT", [128, S], BF16)
            B_kT = [Buf(f"kT{i}") for i in range(NB)]
            vs = sba("vs", [128, NB, 2, 80], BF16)
            B_vs = [Buf(f"vs{i}") for i in range(NB)]
            gT = sba("gT", [128, 16, 128], BF16)
            B_gT = [Buf(f"gT{i}") for i in range(4)]
            et = [sba(f"et{i}", [128, 512], BF16) for i in range(6)]
            B_et = [Buf(f"et{i}") for i in range(6)]
            pT = [t[:].rearrange("p (c q) -> p c q", q=128) for t in et]
            B_pT = B_et
            o_ = sba("o", [128, 512], BF16)
            B_o = Buf("o")
            oT = sba("oT", [128, 4, 128], BF16)
            B_oT = Buf("oT")
            mT = sba("mT", [128, 8, 128], BF16)
            B_mT = [Buf("mTa"), Buf("mTb")]
            B_rstd2 = [Buf("rstd2a"), Buf("rstd2b")]
            h2t = sba("h2t", [128, D], BF16)
            B_h2t = Buf("h2t")
            x1T = sba("x1T", [128, 8, 128], F32)
            B_x1T = Buf("x1T")
            rt = sba("rt", [128, 64], F32)
            affb = [sba(f"affb{i}", [16, 128], F32) for i in range(2)]
            B_affb = [Buf(f"affb{i}") for i in range(2)]

            def stat(i, n=1):
                return st_[:, i:i + n]
            B_s = {k: Buf("st_" + k) for k in ("ss", "tmp", "rstd", "ssv", "tmpv", "rstdv", "ss10", "tmp10", "rstd10",
                                               "den", "rec", "ss2", "tmp2", "rstd2", "se", "rse")}
            SS, TMP, RSTD, SSV, TMPV, RSTDV = stat(0), stat(1), stat(2), stat(3), stat(4), stat(5)
            SS10, TMP10, RSTD10 = stat(8, 10), stat(18, 10), stat(28, 10)
            DEN, REC = stat(38, 8), stat(46, 8)
            SS2, TMP2, RSTD2, SE, RSE = stat(54), stat(55), stat(56), stat(57), stat(58)

            op("pool", lambda e: e.memset(vs[:].rearrange("p b k d -> p (b k d)"), 1.0), writes=B_vs)

            NBT = NSEQ * NB

            def XB(blk):
                return xt[blk % 4], B_xt[blk % 4]

            def ldx(blk):
                X, BX = XB(blk)
                r0 = blk * 128
                dma("sp", lambda e: e.dma_start(out=X[:], in_=x_d[r0:r0 + 128, :]), writes=[BX])

            def pre(blk):
                X, BX = XB(blk)
                hbi = blk % 2
                op("act", lambda e: e.activation(out=junk[0][:], in_=X[:], func=AF.Square, accum_out=SS),
                   reads=[BX], writes=[B_junk[0], B_s["ss"]])
                rstd_from_ss(SS, RSTD, 1, 1.0 / D, B_s["ss"], B_s["rstd"], TMP, B_s["tmp"])
                op("dve", lambda e: e.scalar_tensor_tensor(out=hb[hbi][:], in0=X[:], scalar=RSTD, in1=g1B[:],
                                                           op0=ALU.mult, op1=ALU.mult),
                   reads=[BX, B_s["rstd"], B_W], writes=[B_hb[hbi]])

            def Th(blk):
                par, hbi = blk % 2, blk % 2
                pb = palloc()
                for kc in range(8):
                    op("pe", lambda e, kc=kc: e.transpose(out=pbf(pb)[:, kc * 128:(kc + 1) * 128],
                                                          in_=hb[hbi][:, kc * 128:(kc + 1) * 128], identity=ident_b[:]),
                       reads=[B_hb[hbi], B_ident], writes=[psb[pb]], signal=(kc == 7))
                op("act", lambda e: e.copy(out=hT[par][:].rearrange("p k t -> p (k t)"), in_=pbf(pb)[:, :]),
                   reads=[psb[pb]], writes=[B_hT[par]])

            zbs = {}

            ZP = {"u": (0, 512), "va": (512, 512), "q": (1024, 512), "kv": (1536, 256)}

            def Zs(blk, parts):
                par = blk % 2
                zb = zbs.setdefault(blk, {})
                for nm in parts:
                    c0, w = ZP[nm]
                    b = palloc()
                    zb[nm] = b
                    for kc in range(8):
                        op("pe", lambda e, kc=kc, b=b, c0=c0, w=w: e.matmul(
                            ps[:, b, 0:w], lhsT=hT[par][:, kc, :], rhs=W_in[:, kc, c0:c0 + w],
                            start=(kc == 0), stop=(kc == 7)),
                           reads=[B_hT[par], B_Win[kc]], writes=[psb[b]], signal=(kc == 7))

            def S1e_uv(blk):
                zb = zbs[blk]
                op("act", lambda e: e.activation(out=u[:], in_=ps[:, zb["u"], :], func=AF.Gelu_apprx_tanh),
                   reads=[psb[zb["u"]]], writes=[B_u])
                op("act", lambda e: e.activation(out=va[:], in_=ps[:, zb["va"], :], func=AF.Gelu_apprx_tanh),
                   reads=[psb[zb["va"]]], writes=[B_va])
                op("act", lambda e: e.activation(out=junk[1][:], in_=va[:], func=AF.Square, accum_out=SSV),
                   reads=[B_va], writes=[B_junk[1], B_s["ssv"]])
                rstd_from_ss(SSV, RSTDV, 1, 1.0 / 512, B_s["ssv"], B_s["rstdv"], TMPV, B_s["tmpv"])
                op("dve", lambda e: e.scalar_tensor_tensor(out=vn[:], in0=va[:], scalar=RSTDV, in1=vgB[:],
                                                           op0=ALU.mult, op1=ALU.mult),
                   reads=[B_va, B_s["rstdv"], B_W], writes=[B_vn])

            B_sqk, B_qnk = Buf("sqk"), Buf("qnk")
            B_sk = {k: Buf("st_" + k) for k in ("ssk", "tmpk", "rstdk")}

            def S1e_q1(blk):
                zq = zbs[blk]["q"]
                op("act", lambda e: e.activation(out=sq[:, 0:512], in_=ps[:, zq, :], func=AF.Square),
                   reads=[psb[zq]], writes=[B_sq])
                op("dve", lambda e: e.reduce_sum(out=st_[:, 8:16], in_=sq[:, 0:512].rearrange("p (h d) -> p h d", d=64),
                                                 axis=AX.X), reads=[B_sq], writes=[B_s["ss10"]])
                rstd_from_ss(st_[:, 8:16], st_[:, 28:36], 8, 1.0 / 64, B_s["ss10"], B_s["rstd10"], st_[:, 18:26],
                             B_s["tmp10"])

            def S1e_q2(blk):
                zq = zbs[blk]["q"]
                op("dve", lambda e: e.tensor_tensor(
                    out=qn[:, 0:512].rearrange("p (h d) -> p h d", d=64),
                    in0=ps[:, zq, :].rearrange("p (h d) -> p h d", d=64),
                    in1=st_[:, 28:36].unsqueeze(2).broadcast_to([128, 8, 64]), op=ALU.mult),
                   reads=[psb[zq], B_s["rstd10"]], writes=[B_qn])

            def S1e_k(blk):
                n = blk % NB
                zk = zbs[blk]["kv"]
                op("act", lambda e: e.activation(out=sq[:, 512:640], in_=ps[:, zk, 0:128], func=AF.Square),
                   reads=[psb[zk]], writes=[B_sqk])
                op("act", lambda e: e.copy(out=vs[:, n, :, 0:64],
                                           in_=ps[:, zk, 128:256].rearrange("p (k d) -> p k d", d=64)),
                   reads=[psb[zk]], writes=[B_vs[n]])
                op("dve", lambda e: e.reduce_sum(out=st_[:, 16:18], in_=sq[:, 512:640].rearrange("p (h d) -> p h d", d=64),
                                                 axis=AX.X), reads=[B_sqk], writes=[B_sk["ssk"]])
                rstd_from_ss(st_[:, 16:18], st_[:, 36:38], 2, 1.0 / 64, B_sk["ssk"], B_sk["rstdk"], st_[:, 26:28],
                             B_sk["tmpk"])
                op("dve", lambda e: e.tensor_tensor(
                    out=knt[:].rearrange("p (h d) -> p h d", d=64),
                    in0=ps[:, zk, 0:128].rearrange("p (h d) -> p h d", d=64),
                    in1=st_[:, 36:38].unsqueeze(2).broadcast_to([128, 2, 64]), op=ALU.mult),
                   reads=[psb[zk], B_sk["rstdk"]], writes=[B_knt])
                op("dve", lambda e: e.tensor_tensor(out=qn[:, 512:640], in0=knt[:], in1=gqk[:], op=ALU.mult),
                   reads=[B_knt, B_W], writes=[B_qnk])

            def Tqk_SP(blk):
                n, par = blk % NB, blk % 2
                pb2 = palloc()
                for c in range(5):
                    op("pe", lambda e, c=c: e.transpose(out=pbf(pb2)[:, c * 128:(c + 1) * 128],
                                                        in_=qn[:, c * 128:(c + 1) * 128], identity=ident_b[:]),
                       reads=[B_qn, B_qnk, B_ident], writes=[psb[pb2]], signal=(c == 4))
                op("act", lambda e: e.copy(out=kT[:, n * 128:(n + 1) * 128], in_=pbf(pb2)[:, 512:640]),
                   reads=[psb[pb2]], writes=[B_kT[n]])
                op("act", lambda e: e.copy(out=qT[par][:].rearrange("p k t -> p (k t)"), in_=pbf(pb2)[:, 0:512]),
                   reads=[psb[pb2]], writes=[B_qT[par]])
                pb3 = palloc()
                for g in range(8):
                    op("pe", lambda e, g=g: e.matmul(ps[:, pb3, g * 64:(g + 1) * 64], lhsT=WsT[:, g, :],
                                                     rhs=vn[:, g * 64:(g + 1) * 64], start=True, stop=True),
                       reads=[B_vn, B_Ws], writes=[psb[pb3]], signal=(g == 7))
                for g in range(8):
                    op("dve", lambda e, g=g: e.scalar_tensor_tensor(
                        out=a_[:, g * 64:(g + 1) * 64], in0=ps[:, pb3, g * 64:(g + 1) * 64], scalar=bsp[:, g:g + 1],
                        in1=u[:, g * 64:(g + 1) * 64], op0=ALU.add, op1=ALU.mult),
                       reads=[psb[pb3], B_u, B_bsp], writes=[B_a[g]])

            def Ta(blk):
                par = blk % 2
                pb4 = palloc()
                for c in range(4):
                    op("pe", lambda e, c=c: e.transpose(out=pbf(pb4)[:, c * 128:(c + 1) * 128],
                                                        in_=a_[:, c * 128:(c + 1) * 128], identity=ident_b[:]),
                       reads=[B_a[2 * c], B_a[2 * c + 1], B_ident], writes=[psb[pb4]], signal=(c == 3))
                op("act", lambda e: e.copy(out=aT[par][:].rearrange("p k t -> p (k t)"), in_=pbf(pb4)[:, 0:512]),
                   reads=[psb[pb4]], writes=[B_aT[par]])

            def Gm(blk, q4s=(0, 1, 2, 3)):
                par = blk % 2
                for q4 in q4s:
                    b = palloc()
                    for j in range(4):
                        gc = q4 * 4 + j
                        op("pe", lambda e, b=b, j=j, gc=gc: e.matmul(
                            ps[:, b, j * 128:(j + 1) * 128], lhsT=bgrow[:, gc * 128:(gc + 1) * 128], rhs=onesr[:, :],
                            start=True, stop=False), reads=[B_W, B_bg], writes=[psb[b]], signal=False)
                        for kc in range(8):
                            op("pe", lambda e, kc=kc, b=b, j=j, gc=gc: e.matmul(
                                ps[:, b, j * 128:(j + 1) * 128],
                                lhsT=W_in[:, kc, 1792 + gc * 128:1792 + (gc + 1) * 128], rhs=hT[par][:, kc, :],
                                start=False, stop=(kc == 7)),
                               reads=[B_hT[par], B_Wing[kc]], writes=[psb[b]], signal=(kc == 7 and j == 3))
                    op("act", lambda e, b=b, q4=q4: e.activation(
                        out=gT[:, q4 * 4:(q4 + 1) * 4, :].rearrange("p g t -> p (g t)"), in_=ps[:, b, :], func=AF.Tanh,
                        scale=0.5), reads=[psb[b]], writes=[B_gT[q4]])

            gTf = gT[:].rearrange("p g t -> p (g t)")

            def PAm(blk):
                par = blk % 2
                pab = palloc(2)
                for dc in range(8):
                    for kc in range(4):
                        op("pe", lambda e, dc=dc, kc=kc: e.matmul(
                            ps[:, pab + dc // 4, (dc % 4) * 128:(dc % 4 + 1) * 128],
                            lhsT=Wpa[:, kc, dc * 128:(dc + 1) * 128], rhs=aT[par][:, kc, :],
                            start=(kc == 0), stop=(kc == 3)),
                           reads=[B_aT[par], B_Wp[kc]], writes=[psb[pab + dc // 4]], signal=(kc == 3 and dc % 4 == 3))
                op("dve", lambda e: e.scalar_tensor_tensor(
                    out=t1[:], in0=gTf[:, 0:1024], scalar=1.0, in1=ps[:, pab:pab + 2, :].rearrange("p k c -> p (k c)"),
                    op0=ALU.add, op1=ALU.mult), reads=[B_gT[0], B_gT[1], psb[pab], psb[pab + 1]], writes=[B_t1])

            def SCm(blk):
                m, par = blk % NB, blk % 2
                kbs = [kb for kb in range(3) if 0 <= m + kb - 1 < NB]
                for kv in range(2):
                    for kb in kbs:
                        kblk = m + kb - 1
                        b = palloc()
                        ei = kv * 3 + kb
                        op("pe", lambda e, b=b, kv=kv, kblk=kblk: e.matmul(
                            ps[:, b, :], lhsT=kT[kv * 64:(kv + 1) * 64, kblk * 128:(kblk + 1) * 128],
                            rhs=qT[par][kv * 64:(kv + 1) * 64, :, :], start=True, stop=True),
                           reads=[B_kT[kblk], B_qT[par]], writes=[psb[b]], signal=True)
                        op("act", lambda e, b=b, ei=ei: e.activation(out=et[ei][:], in_=ps[:, b, :], func=AF.Exp),
                           reads=[psb[b]], writes=[B_et[ei]])
                        op("dve", lambda e, kb=kb, kv=kv, ei=ei: e.tensor_tensor(
                            out=et[ei][:], in0=et[ei][:],
                            in1=expb[:, (kb * 8 + kv * 4) * 128:(kb * 8 + kv * 4 + 4) * 128], op=ALU.mult),
                           reads=[B_eb], writes=[B_et[ei]])

            def PVm(blk):
                m = blk % NB
                kbs = [kb for kb in range(3) if 0 <= m + kb - 1 < NB]
                ob = palloc(2)
                for kv in range(2):
                    for c in range(4):
                        for i, kb in enumerate(kbs):
                            kblk = m + kb - 1
                            ei = kv * 3 + kb
                            op("pe", lambda e, kv=kv, c=c, ei=ei, kblk=kblk, i=i: e.matmul(
                                ps[:, ob + kv, c * 65:(c + 1) * 65], lhsT=pT[ei][:, c, :], rhs=vs[:, kblk, kv, 0:65],
                                start=(i == 0), stop=(i == len(kbs) - 1)),
                               reads=[B_pT[ei], B_vs[kblk]], writes=[psb[ob + kv]],
                               signal=(c == 3 and i == len(kbs) - 1))
                ov = ps[:, ob:ob + 2, 0:260].rearrange("p k (c d) -> p k c d", d=65)
                op("dve", lambda e: e.tensor_tensor(out=DEN.rearrange("p (k c) -> p k c", c=4), in0=ov[:, :, :, 64],
                                                    in1=esink[:].rearrange("p (k c) -> p k c", c=4), op=ALU.add),
                   reads=[psb[ob], psb[ob + 1], B_W], writes=[B_s["den"]])
                op("dve", lambda e: e.reciprocal(out=REC, in_=DEN), reads=[B_s["den"]], writes=[B_s["rec"]])
                op("dve", lambda e: e.tensor_tensor(
                    out=o_[:].rearrange("p (k c d) -> p k c d", c=4, d=64), in0=ov[:, :, :, 0:64],
                    in1=st_[:, 46:54].rearrange("p (k c) -> p k c", c=4).unsqueeze(3).broadcast_to([128, 2, 4, 64]),
                    op=ALU.mult), reads=[psb[ob], psb[ob + 1], B_s["rec"]], writes=[B_o])

            def Tom(blk):
                pb = palloc()
                for c in range(4):
                    op("pe", lambda e, c=c: e.transpose(out=pbf(pb)[:, c * 128:(c + 1) * 128],
                                                        in_=o_[:, c * 128:(c + 1) * 128], identity=ident_b[:]),
                       reads=[B_o, B_ident], writes=[psb[pb]], signal=(c == 3))
                op("act", lambda e: e.copy(out=oT[:].rearrange("p k t -> p (k t)"), in_=pbf(pb)[:, 0:512]),
                   reads=[psb[pb]], writes=[B_oT])

            def PB_Y(blk):
                X, BX = XB(blk)
                r0 = blk * 128
                pbb = palloc(2)
                for hf in range(2):
                    for dc in range(hf * 4, hf * 4 + 4):
                        for kc in range(4):
                            op("pe", lambda e, dc=dc, kc=kc: e.matmul(
                                ps[:, pbb + dc // 4, (dc % 4) * 128:(dc % 4 + 1) * 128],
                                lhsT=Wpb[:, kc, dc * 128:(dc + 1) * 128], rhs=oT[:, kc, :],
                                start=(kc == 0), stop=(kc == 3)),
                               reads=[B_oT, B_Wp[4 + kc]], writes=[psb[pbb + hf]], signal=(kc == 3 and dc % 4 == 3))
                    op("dve", lambda e, hf=hf: e.scalar_tensor_tensor(
                        out=t2[:, hf * 512:(hf + 1) * 512], in0=gTf[:, 1024 + hf * 512:1024 + (hf + 1) * 512], scalar=1.0,
                        in1=ps[:, pbb + hf, :], op0=ALU.add, op1=ALU.mult),
                       reads=[B_gT[2 + hf], psb[pbb + hf]], writes=[B_t2[hf]])
                    op("dve", lambda e, hf=hf: e.tensor_tensor(
                        out=mT[:].rearrange("p k t -> p (k t)")[:, hf * 512:(hf + 1) * 512],
                        in0=t1[:, hf * 512:(hf + 1) * 512], in1=t2[:, hf * 512:(hf + 1) * 512], op=ALU.add),
                       reads=[B_t1, B_t2[hf]], writes=[B_mT[hf]])

            def Ym(blk):
                X, BX = XB(blk)
                r0 = blk * 128
                yb = palloc(2)
                for hf in range(2):
                    for ch in range(2):
                        for kc in range(hf * 4, hf * 4 + 4):
                            op("pe", lambda e, ch=ch, kc=kc: e.matmul(
                                ps[:, yb + ch, :], lhsT=mT[:, kc, :], rhs=Wout[:, kc, ch * 512:(ch + 1) * 512],
                                start=(kc == 0), stop=(kc == 7), skip_group_check=True),
                               reads=[B_mT[hf], B_Wo[kc]], writes=[psb[yb + ch]], signal=(kc == 7))
                op("dve", lambda e: e.scalar_tensor_tensor(
                    out=X[:], in0=ps[:, yb:yb + 2, :].rearrange("p k c -> p (k c)"), scalar=0.5, in1=X[:],
                    op0=ALU.mult, op1=ALU.add), reads=[psb[yb], psb[yb + 1], BX], writes=[BX])
                dma("sp", lambda e: e.dma_start(out=out_d[r0:r0 + 128, :], in_=X[:]), reads=[BX], mwrites=[B_out[blk // NB]])
                op("act", lambda e: e.activation(out=junk[2][:], in_=X[:], func=AF.Square, accum_out=SS2),
                   reads=[BX], writes=[B_junk[2], B_s["ss2"]])
                rstd_from_ss(SS2, stat(59 + blk % 2), 1, 1.0 / D, B_s["ss2"], B_rstd2[blk % 2], TMP2, B_s["tmp2"])
                op("dve", lambda e: e.scalar_tensor_tensor(out=h2t[:], in0=X[:], scalar=stat(59 + blk % 2), in1=g2B[:],
                                                           op0=ALU.mult, op1=ALU.mult),
                   reads=[BX, B_rstd2[blk % 2], B_W], writes=[B_h2t])
                dma("sp", lambda e: e.dma_start(out=h2_d[r0:r0 + 128, :], in_=h2t[:]), reads=[B_h2t],
                    mwrites=[B_h2d[blk // NB]])

            def Rt_a(blk):
                X, BX = XB(blk)
                s, m = divmod(blk, NB)
                R2 = stat(59 + blk % 2)
                xb = palloc(2)
                for kc in range(8):
                    op("pe", lambda e, kc=kc: e.transpose(out=ps[:, xb + kc // 4, (kc % 4) * 128:(kc % 4 + 1) * 128],
                                                          in_=X[:, kc * 128:(kc + 1) * 128], identity=ident_f[:]),
                       reads=[BX, B_ident], writes=[psb[xb + kc // 4]], signal=(kc % 4 == 3))
                op("act", lambda e: e.copy(out=x1T[:].rearrange("p k t -> p (k t)"),
                                           in_=ps[:, xb:xb + 2, :].rearrange("p k c -> p (k c)")),
                   reads=[psb[xb], psb[xb + 1]], writes=[B_x1T])

            lbs = {}

            def Rt_b(blk):
                R2 = stat(59 + blk % 2)
                lb = palloc()
                lbs[blk] = lb
                for kc in range(8):
                    op("pe", lambda e, kc=kc: e.matmul(ps[:, lb, 0:E], lhsT=x1T[:, kc, :], rhs=Wr[:, kc, :],
                                                       start=(kc == 0), stop=(kc == 7)),
                       reads=[B_x1T, B_Wr], writes=[psb[lb]], signal=(kc == 7))
                B_rt = B_s["se"]
                op("act", lambda e: e.activation(out=rt[:, 0:E], in_=ps[:, lb, 0:E], func=AF.Exp, scale=R2,
                                                 accum_out=SE),
                   reads=[psb[lb], B_rstd2[blk % 2]], writes=[B_rt])
                op("dve", lambda e: e.reciprocal(out=RSE, in_=SE), reads=[B_rt], writes=[B_s["rse"]])
                op("dve", lambda e: e.tensor_scalar(out=rt[:, 16:32], in0=rt[:, 0:E], scalar1=RSE, scalar2=None,
                                                    op0=ALU.mult), reads=[B_rt, B_s["rse"]], writes=[B_s["rse"]])

            def Rt_c(blk):
                s, m = divmod(blk, NB)
                ab = palloc()
                op("pe", lambda e: e.transpose(out=ps[0:E, ab, 0:128], in_=rt[:, 16:32], identity=ident_f[:]),
                   reads=[B_s["rse"], B_ident], writes=[psb[ab]], signal=True)
                af = affb[blk % 2]
                op("act", lambda e: e.copy(out=af[:], in_=ps[0:E, ab, 0:128]), reads=[psb[ab]], writes=[B_affb[blk % 2]])
                hf_, mm_ = divmod(m, NB // 2)
                dma("sp", lambda e: e.dma_start(out=aff_all[hf_ * 64 + s * E:hf_ * 64 + (s + 1) * E,
                                                            mm_ * 128:(mm_ + 1) * 128], in_=af[:]),
                    reads=[B_affb[blk % 2]], mwrites=[B_aff_all])

            ldx(0)
            if NBT > 1:
                ldx(1)
            pre(0)
            Th(0)
            Zs(0, ("q", "kv"))
            S1e_q1(0)
            S1e_q2(0)
            S1e_k(0)
            for c3 in range(3):
                dma("sp", lambda e, c3=c3: e.dma_start(out=t1[:], in_=bias_d[:, c3 * 1024:(c3 + 1) * 1024]), writes=[B_t1])
                dma("sp", lambda e, c3=c3: e.dma_start(out=t2[:], in_=mask_d[:, c3 * 1024:(c3 + 1) * 1024]), writes=B_t2)
                op("act", lambda e: e.activation(out=t1[:], in_=t1[:], func=AF.Exp), reads=[B_t1], writes=[B_t1])
                op("dve", lambda e, c3=c3: e.tensor_tensor(out=expb[:, c3 * 1024:(c3 + 1) * 1024], in0=t1[:], in1=t2[:],
                                                           op=ALU.mult), reads=[B_t1] + B_t2, writes=[B_eb])
            for i in range(NBT + 2):
                n, m, r = i, i - 1, i - 2
                hn, hm, hr, hn1 = n < NBT, 0 <= m < NBT, 0 <= r < NBT, n + 1 < NBT
                if hn:
                    Zs(n, ("u", "va"))
                    S1e_uv(n)
                if hn1:
                    pre(n + 1)
                if hm:
                    Gm(m, (0, 1))
                if hr:
                    Rt_a(r)
                if n + 2 < NBT:
                    ldx(n + 2)
                if hm:
                    PAm(m)
                if hn:
                    Tqk_SP(n)
                    zbs.pop(n)
                if hr:
                    Rt_b(r)
                if hm:
                    SCm(m)
                    Gm(m, (2,))
                    PVm(m)
                    Gm(m, (3,))
                if hn1:
                    Th(n + 1)
                if hr:
                    Rt_c(r)
                if hm:
                    Tom(m)
                if hn1:
                    Zs(n + 1, ("q",))
                    S1e_q1(n + 1)
                if hm:
                    PB_Y(m)
                if hn1:
                    S1e_q2(n + 1)
                    Zs(n + 1, ("kv",))
                    S1e_k(n + 1)
                if hn:
                    Ta(n)
                if hm:
                    Ym(m)
                if i == min(1, NBT - 1):
                    for kc in range(8):
                        op("dve", lambda e, kc=kc: e.tensor_scalar(out=Wr[:, kc, :], in0=Wr[:, kc, :], scalar1=g2c[:, kc:kc + 1],
                                                                   scalar2=None, op0=ALU.mult), reads=[B_Wr], writes=[B_Wr])

            fin = Buf("finA")
            allb = ([B_W, B_bg, B_Ws, B_Wr, B_eb, B_bsp, B_u, B_va, B_vn, B_sq, B_sqk, B_qn, B_qnk, B_knt, B_o, B_oT, B_t1,
                     B_h2t, B_x1T] + B_a + B_Wing + B_Win + B_Wp + B_Wo + B_junk + B_hb + B_gT + B_t2 + B_mT + B_rstd2 + B_xt + B_hT + B_aT + B_qT + B_kT
                    + B_vs + B_et + B_affb + list(B_s.values()) + list(B_sk.values()) + psb)
            op("dve", lambda e: e.memset(rt[:, 32:40], 0.0), reads=[], writes=allb + [fin])

        NSE = NSEQ * E
        for ek in ("pe", "act", "pool", "sp", "dve"):
            dp._deps(ek, [fin], [])
        if dbg:
            dma("sp", lambda e: e.dma_start(out=aff_dbg[:, :], in_=aff_all[:, :]), reads=[B_aff_all, fin])
        NR = 3
        if upto == "C":
            WgQ = [sb(f"WgQ{i}", [128, 8, 512], BF16) for i in range(NR)]
            WuQ = [sb(f"WuQ{i}", [128, 8, 512], BF16) for i in range(NR)]
            WdQ = [sb(f"WdQ{i}", [128, 4, D], BF16) for i in range(NR)]
            B_WgQ = [Buf(f"WgQ{i}") for i in range(NR)]
            B_WuQ = [Buf(f"WuQ{i}") for i in range(NR)]
            B_WdQ = [Buf(f"WdQ{i}") for i in range(NR)]
            xg = [sb(f"xg{i}", [128, D], BF16) for i in range(2 * NSEQ)]
            B_xg = [Buf(f"xg{i}") for i in range(2 * NSEQ)]
            xgT = [sb(f"xgT{i}", [128, 8, NSEQ * CAP], BF16) for i in range(2)]
            B_xgT = [[Buf(f"xgT{i}_{s}") for s in range(NSEQ)] for i in range(2)]
            yacc = [sb(f"yacc{s}", [128, 2, D], F32) for s in range(NSEQ)]
            B_yacc = [Buf(f"yacc{s}") for s in range(NSEQ)]
            sg = [sb(f"sg{i}", [128, 512], F32) for i in range(2)]
            B_sg = [Buf(f"sg{i}") for i in range(2)]
            hid = [sb(f"hid{i}", [128, 512], BF16) for i in range(8)]
            B_hid = [Buf(f"hid{i}") for i in range(8)]
            rgc = [0]

            def palloc_c():
                rgc[0] = (rgc[0] + 1) % 4
                return 4 + rgc[0]

            def load_chunk(k):
                e_, Q = divmod(k, 4)
                sl = k % NR
                c0 = Q * 512
                dma("pool", lambda e: e.dma_start(
                    out=WgQ[sl][:], in_=wge_d[e_, :, c0:c0 + 512].rearrange("(k p) c -> p k c", p=128)),
                    writes=[B_WgQ[sl]])
                dma("pool", lambda e: e.dma_start(
                    out=WuQ[sl][:], in_=wue_d[e_, :, c0:c0 + 512].rearrange("(k p) c -> p k c", p=128)),
                    writes=[B_WuQ[sl]])
                dma("pool", lambda e: e.dma_start(
                    out=WdQ[sl][:], in_=wde_d[e_, c0:c0 + 512, :].rearrange("(k p) c -> p k c", p=128)),
                    writes=[B_WdQ[sl]])

            def prefetch_c():
                load_chunk(0)
                load_chunk(1)
                load_chunk(2)

            def gathers(e_):
                for s in range(NSEQ):
                    se = s * E + e_
                    for h in range(2):
                        xi = s * 2 + h
                        dma("pool", lambda e, h=h, xi=xi, se=se: e.indirect_dma_start(
                            out=xg[xi][:], out_offset=None, in_=h2_d[:, :],
                            in_offset=bass.IndirectOffsetOnAxis(ap=idxT[:, h * 64 + se:h * 64 + se + 1], axis=0)),
                            reads=[B_idxT] + B_h2d, writes=[B_xg[xi]])

            def xg_transposes(e_, s):
                par = e_ % 2
                for h in range(2):
                    xi = s * 2 + h
                    tb = palloc_c()
                    for kc in range(8):
                        op("pe", lambda e, kc=kc, xi=xi, tb=tb: e.transpose(
                            out=pbf(tb)[:, kc * 128:(kc + 1) * 128], in_=xg[xi][:, kc * 128:(kc + 1) * 128],
                            identity=ident_b[:]), reads=[B_xg[xi], B_ident], writes=[psb[tb]], signal=(kc == 7))
                    op("act", lambda e, h=h, tb=tb: e.copy(
                        out=xgT[par][:, :, s * CAP + h * 128:s * CAP + (h + 1) * 128],
                        in_=pbf(tb)[:, :].rearrange("p (k t) -> p k t", t=128)),
                       reads=[psb[tb]], writes=[B_xgT[par][s]])

        idxT = sb("idxT", [128, 2 * 64], U32)
        gatT = sb("gatT", [128, 2 * 64], F32)
        B_idxT = Buf("idxT")
        B_gatT = Buf("gatT")
        if upto in ("B", "C"):
            op("dve", lambda e: e.memset(idxT[:], 0), reads=[fin], writes=[B_idxT])
            op("dve", lambda e: e.memset(gatT[:], 0.0), reads=[fin], writes=[B_gatT])
            if upto == "C":
                prefetch_c()
            if True:
                HS = S // 2
                vals = sb("vals", [128, CAP], F32)
                idxu = sb("idxu", [128, CAP], U32)
                idxf = sb("idxf", [128, CAP], F32)
                work = sb("work", [128, HS], F32)
                offs = sb("offs_sb", [128, 1], F32)
                vT = sb("vT_sb", [128, 2, 128], F32)
                iT = sb("iT_sb", [128, 2, 128], F32)
                mrg = sb("mrg", [128, 3, 128], F32)
                B_vals, B_idxu, B_idxf, B_work, B_offs = Buf("vals"), Buf("idxu"), Buf("idxf"), Buf("work"), Buf("offs")
                B_vT, B_iT, B_mrg = Buf("vT"), Buf("iT"), Buf("mrg")
                dma("sp", lambda e: e.dma_start(out=offs[:], in_=offs_d[:, :]), writes=[B_offs])
                cur, Bcur = aff_all, B_aff_all
                for r in range(CAP // 8):
                    op("dve", lambda e, r=r, cur=cur: e.max(out=vals[:, r * 8:(r + 1) * 8], in_=cur[:, :]),
                       reads=[Bcur, fin], writes=[B_vals])
                    op("dve", lambda e, r=r, cur=cur: e.max_index(out=idxu[:, r * 8:(r + 1) * 8],
                                                                  in_max=vals[:, r * 8:(r + 1) * 8],
                                                                  in_values=cur[:, :]),
                       reads=[Bcur, B_vals], writes=[B_idxu])
                    if r < CAP // 8 - 1:
                        op("dve", lambda e, r=r, cur=cur: e.match_replace(
                            out=work[:, :], in_to_replace=vals[:, r * 8:(r + 1) * 8], in_values=cur[:, :],
                            imm_value=-1.0), reads=[Bcur, B_vals], writes=[B_work])
                        cur, Bcur = work, B_work
                op("dve", lambda e: e.tensor_copy(out=idxf[:, :], in_=idxu[:, :]), reads=[B_idxu], writes=[B_idxf])
                op("dve", lambda e: e.tensor_scalar(out=idxf[:, :], in0=idxf[:, :], scalar1=offs[:, 0:1],
                                                    scalar2=None, op0=ALU.add), reads=[B_idxf, B_offs], writes=[B_idxf])
                tb, tb2 = palloc(), palloc()
                for h in range(2):
                    op("pe", lambda e, h=h: e.transpose(out=ps[:, tb, h * 128:(h + 1) * 128],
                                                        in_=vals[:, h * 128:(h + 1) * 128], identity=ident_f[:]),
                       reads=[B_vals, B_ident], writes=[psb[tb]], signal=(h == 1))
                for h in range(2):
                    op("pe", lambda e, h=h: e.transpose(out=ps[:, tb2, h * 128:(h + 1) * 128],
                                                        in_=idxf[:, h * 128:(h + 1) * 128], identity=ident_f[:]),
                       reads=[B_idxf, B_ident], writes=[psb[tb2]], signal=(h == 1))
                op("dve", lambda e: e.tensor_copy(out=vT[:].rearrange("p h r -> p (h r)"), in_=ps[:, tb, 0:256]),
                   reads=[psb[tb]], writes=[B_vT])
                op("dve", lambda e: e.tensor_copy(out=iT[:].rearrange("p h r -> p (h r)"), in_=ps[:, tb2, 0:256]),
                   reads=[psb[tb2]], writes=[B_iT])
                fb, fb2 = palloc(), palloc()
                for h in range(2):
                    op("pe", lambda e, h=h: e.matmul(ps[:, fb, h * 64:(h + 1) * 64], lhsT=flipm[:],
                                                     rhs=vT[:, 1 - h, 64:128], start=True, stop=True),
                       reads=[B_vT, B_ident], writes=[psb[fb]], signal=(h == 1))
                for h in range(2):
                    op("pe", lambda e, h=h: e.matmul(ps[:, fb2, h * 64:(h + 1) * 64], lhsT=flipm[:],
                                                     rhs=iT[:, 1 - h, 64:128], start=True, stop=True),
                       reads=[B_iT, B_ident], writes=[psb[fb2]], signal=(h == 1))
                A3 = vT[:, :, 0:64]
                I3 = iT[:, :, 0:64]
                Br3 = ps[:, fb, 0:128].rearrange("p (h r) -> p h r", r=64)
                Ir3 = ps[:, fb2, 0:128].rearrange("p (h r) -> p h r", r=64)
                g3 = gatT[:].rearrange("p (h r) -> p h r", r=64)
                op("dve", lambda e: e.tensor_tensor(out=g3, in0=A3, in1=Br3, op=ALU.max),
                   reads=[B_vT, psb[fb]], writes=[B_gatT])
                op("dve", lambda e: e.tensor_tensor(out=mrg[:, 0, :].rearrange("p (h r) -> p h r", r=64), in0=A3, in1=Br3,
                                                    op=ALU.is_ge), reads=[B_vT, psb[fb]], writes=[B_mrg])
                op("dve", lambda e: e.tensor_tensor(out=mrg[:, 1, :].rearrange("p (h r) -> p h r", r=64), in0=I3, in1=Ir3,
                                                    op=ALU.subtract), reads=[B_iT, psb[fb2], B_mrg], writes=[B_mrg])
                op("dve", lambda e: e.tensor_tensor(out=mrg[:, 2, :], in0=mrg[:, 1, :], in1=mrg[:, 0, :], op=ALU.mult),
                   reads=[B_mrg], writes=[B_mrg])
                op("dve", lambda e: e.tensor_tensor(out=mrg[:, 1, :], in0=mrg[:, 2, :], in1=ps[:, fb2, 0:128], op=ALU.add),
                   reads=[B_mrg, psb[fb2]], writes=[B_mrg])
                op("dve", lambda e: e.tensor_copy(out=idxT[:], in_=mrg[:, 1, :]), reads=[B_mrg], writes=[B_idxT])
                finB = Buf("finB")
                op("dve", lambda e: e.memset(offs[:], 0.0),
                   reads=[B_idxT, B_gatT], writes=[B_vals, B_idxu, B_idxf, B_work, B_offs, B_vT, B_iT, B_mrg,
                                                   psb[tb], psb[tb2], psb[fb], psb[fb2], finB])
            if dbg:
                dma("sp", lambda e: e.dma_start(out=idx_dbg[:, :], in_=idxT[:]), reads=[B_idxT, finB])
                dma("sp", lambda e: e.dma_start(out=gat_dbg[:, :], in_=gatT[:]), reads=[B_gatT, finB])

        if upto == "C":
            NP = NSEQ // 2
            NCH = E * 4
            units = [(k, sp) for k in range(NCH) for sp in range(NP)]

            def GU2(k, sp, up_, js):
                e_, Q = divmod(k, 4)
                sl, par = k % NR, e_ % 2
                for j in js:
                    ga, gu_ = palloc_c(), palloc_c()
                    rhs = lambda kc: xgT[par][:, kc, sp * 512:(sp + 1) * 512]
                    for kc in range(8):
                        op("pe", lambda e, kc=kc: e.matmul(
                            ps[:, ga, :], lhsT=WgQ[sl][:, kc, j * 128:(j + 1) * 128], rhs=rhs(kc),
                            start=(kc == 0), stop=(kc == 7)),
                           reads=[B_WgQ[sl], B_xgT[par][2 * sp], B_xgT[par][2 * sp + 1]], writes=[psb[ga]], signal=(kc == 7))
                    for kc in range(8):
                        op("pe", lambda e, kc=kc: e.matmul(
                            ps[:, gu_, :], lhsT=WuQ[sl][:, kc, j * 128:(j + 1) * 128], rhs=rhs(kc),
                            start=(kc == 0), stop=(kc == 7)),
                           reads=[B_WuQ[sl], B_xgT[par][2 * sp], B_xgT[par][2 * sp + 1]], writes=[psb[gu_]], signal=(kc == 7))
                    sgi = j % 2
                    hi = up_ * 4 + j
                    op("act", lambda e, sgi=sgi: e.activation(out=sg[sgi][:], in_=ps[:, ga, :], func=AF.Silu),
                       reads=[psb[ga]], writes=[B_sg[sgi]])
                    op("dve", lambda e, sgi=sgi, hi=hi: e.tensor_tensor(
                        out=hid[hi][:], in0=sg[sgi][:], in1=ps[:, gu_, :], op=ALU.mult),
                       reads=[B_sg[sgi], psb[gu_]], writes=[B_hid[hi]])

            def DN(k, sp, up_, sl_):
                e_, Q = divmod(k, 4)
                sl = k % NR
                s = 2 * sp + sl_
                se = s * E + e_
                for st in range(2):
                    for dh in range(2):
                        for j in range(4):
                            hi = up_ * 4 + j
                            op("pe", lambda e, st=st, dh=dh, j=j, hi=hi: e.matmul(
                                ps[:, st * 2 + dh, :], lhsT=hid[hi][:, sl_ * 256 + st * 128:sl_ * 256 + (st + 1) * 128],
                                rhs=WdQ[sl][:, j, dh * 512:(dh + 1) * 512], start=(j == 0), stop=(j == 3)),
                               reads=[B_hid[hi], B_WdQ[sl]], writes=[psb[st * 2 + dh]], signal=(j == 3 and dh == 1))
                for st in range(2):
                    b0 = st * 2
                    src = ps[:, b0:b0 + 2, :].rearrange("p k c -> p (k c)")
                    gcol = gatT[:, st * 64 + se:st * 64 + se + 1]
                    if Q == 0:
                        op("act", lambda e, st=st, src=src, gcol=gcol: e.activation(
                            out=yacc[s][:, st, :], in_=src, func=AF.Copy, scale=gcol),
                           reads=[psb[b0], psb[b0 + 1], B_gatT], writes=[B_yacc[s]])
                    else:
                        op("dve", lambda e, st=st, src=src, gcol=gcol: e.scalar_tensor_tensor(
                            out=yacc[s][:, st, :], in0=src, scalar=gcol, in1=yacc[s][:, st, :],
                            op0=ALU.mult, op1=ALU.add),
                           reads=[psb[b0], psb[b0 + 1], B_gatT, B_yacc[s]], writes=[B_yacc[s]])
                if Q == 3:
                    for st in range(2):
                        dma("pool", lambda e, st=st: e.indirect_dma_start(
                            out=out_d[:, :], out_offset=bass.IndirectOffsetOnAxis(
                                ap=idxT[:, st * 64 + se:st * 64 + se + 1], axis=0),
                            in_=yacc[s][:, st, :], in_offset=None, compute_op=ALU.add),
                            reads=[B_yacc[s], B_idxT], writes=[B_out[s]])

            gathers(0)
            for s in range(NSEQ):
                xg_transposes(0, s)
            for ui, (k, sp) in enumerate(units):
                e_, Q = divmod(k, 4)
                GU2(k, sp, ui % 2, (0, 1))
                if ui >= 1:
                    pk, psp = units[ui - 1]
                    DN(pk, psp, (ui - 1) % 2, 0)
                GU2(k, sp, ui % 2, (2, 3))
                if ui >= 1:
                    DN(pk, psp, (ui - 1) % 2, 1)
                if sp == 0 and k >= 1 and k + 2 < NCH:
                    load_chunk(k + 2)
                if Q == 1 and sp == 0 and e_ + 1 < E:
                    gathers(e_ + 1)
                if Q == 3 and e_ + 1 < E:
                    xg_transposes(e_ + 1, 2 * sp)
                    xg_transposes(e_ + 1, 2 * sp + 1)
            lk, lsp = units[-1]
            DN(lk, lsp, (len(units) - 1) % 2, 0)
            DN(lk, lsp, (len(units) - 1) % 2, 1)
            finC = Buf("finC")
            allc = (B_WgQ + B_WuQ + B_WdQ + B_xg + B_xgT[0] + B_xgT[1] + B_yacc + B_sg + B_hid + psb)
            op("dve", lambda e: e.memset(neghalf[:, 0:1], 0.0), reads=[], writes=allc + [B_const, finC])

        tail = B_out + B_h2d + ([B_aff_all, B_idxT, B_gatT] if dbg else [])
        dp._deps("sp", tail, tail)
        dp._deps("pool", tail, tail)
    return nc


def _host_tables(rel_bias_table):
    j = np.arange(128)[:, None, None]
    kb = np.arange(3)[None, :, None]
    q = np.arange(128)[None, None, :]
    rel = (kb - 1) * 128 + j - q
    bucket = _t5_bucket(rel)
    bias = rel_bias_table[bucket]
    bias = np.ascontiguousarray(bias.transpose(0, 1, 3, 2)).reshape(128, 3 * 8 * 128).astype(np.float32)
    mask = (np.abs(rel) <= 128).astype(np.float32)
    mask = np.ascontiguousarray(np.broadcast_to(mask[:, :, None, :], (128, 3, 8, 128))).reshape(128, 3 * 8 * 128)
    return bias, mask


def make_in_maps(inputs, ncores, nseq, edecl=E):
    f = lambda k: np.ascontiguousarray(np.asarray(inputs[k], dtype=np.float32))
    x = f("x")
    w_in = f("w_in")[0]
    qcols = np.concatenate([np.arange(1024 + h * 64, 1024 + (h + 1) * 64) for h in QPERM])
    cols = np.concatenate([np.arange(0, 1024), qcols, np.arange(1536, IN_COLS)])
    w_in_p = np.ascontiguousarray(w_in[:, cols])
    bias, mask = _host_tables(f("rel_bias_table"))
    common = {
        "norm_mix_g": f("norm_mix_g")[0], "w_in": w_in_p, "b_gate": f("b_gate")[0], "vnorm_g": f("vnorm_g")[0],
        "w_spatial": f("w_spatial")[0], "b_spatial": f("b_spatial")[0], "q_norm_g": f("q_norm_g")[0],
        "k_norm_g": f("k_norm_g")[0], "attn_sink": f("attn_sink")[0], "bias_exp": bias, "band_mask": mask,
        "w_proj_a": f("w_proj_a")[0], "w_proj_b": f("w_proj_b")[0], "w_out": f("w_out")[0],
        "norm_ffn_g": f("norm_ffn_g")[0], "w_router": f("w_router")[0], "w_gate_e": f("w_gate_e")[0][:edecl],
        "w_up_e": f("w_up_e")[0][:edecl], "w_down_e": f("w_down_e")[0][:edecl],
        "ident": np.eye(128, dtype=np.float32),
        "offs": (2048.0 * ((np.arange(128) % 64) // 16) + 1024.0 * (np.arange(128) // 64)).astype(np.float32).reshape(128, 1),
        "flip": np.ascontiguousarray(np.eye(128, dtype=np.float32)[::-1]),
    }
    maps = []
    for c in range(ncores):
        m = dict(common)
        m["x"] = np.ascontiguousarray(x[c * nseq:(c + 1) * nseq].reshape(nseq * S, D))
        maps.append(m)
    return maps


_NC_CACHE = {}


def kernel(**inputs):
    nseq = 4
    if "full" not in _NC_CACHE:
        _NC_CACHE["full"] = build(NSEQ=nseq, upto="C", dbg=False)
    nc = _NC_CACHE["full"]
    in_maps = make_in_maps(inputs, NCORES, nseq)
    res = run_bass_kernel_spmd(nc, in_maps, core_ids=list(range(NCORES)))
    out = np.concatenate([np.asarray(r["out"]).reshape(nseq, S, D) for r in res.results], axis=0)
    return out.astype(np.float32)
```

```python
import contextlib
import numpy as np
import concourse.bass as bass
import concourse.mybir as mybir
from concourse.bass_utils import run_bass_kernel_spmd

F32, BF16, U32 = mybir.dt.float32, mybir.dt.bfloat16, mybir.dt.uint32
AF = mybir.ActivationFunctionType
ALU = mybir.AluOpType
AX = mybir.AxisListType

D = 1024
S = 2048
NB = S // 128
E = 16
FF = 2048
CAP = 256
IN_COLS = 3840
EPS = 1e-6
NCORES = 8
CUT = 99


class Buf:
    __slots__ = ("name", "w", "r", "wm")

    def __init__(self, name=""):
        self.name = name
        self.w = None
        self.r = {}
        self.wm = {}


class Dep:
    def __init__(self, nc, es, ndma=8):
        self.nc = nc
        self.E = dict(pe=nc.tensor, dve=nc.vector, act=nc.scalar, pool=nc.gpsimd, sp=nc.sync)
        self.csem = {k: es.enter_context(nc.semaphore("c_" + k)) for k in ("pe", "dve", "act", "pool")}
        self.ccnt = {k: 0 for k in self.csem}
        self.seen = {k: {} for k in self.E}
        self.pend = {k: [] for k in self.csem}
        self.dsem = {q: [es.enter_context(nc.semaphore(f"d_{q}{i}")) for i in range(ndma)] for q in ("sp", "pool")}
        self.dval = {q: [0] * ndma for q in self.dsem}
        self.dpos = {q: 0 for q in self.dsem}
        self.ndma = ndma

    def wait(self, ek, ev):
        key, sem, val = ev
        if ek == "pe" and key == "c_pe":
            return
        if self.seen[ek].get(key, 0) >= val:
            return
        self.E[ek].wait_ge(sem, val)
        self.seen[ek][key] = val

    def _deps(self, ek, reads, writes, mwrites=()):
        for b in reads:
            assert not any(pb is b and md == "w" for pe_ in self.pend.values() for (pb, md) in pe_), \
                f"read of unsignaled buffer {b.name}"
            if b.w is not None:
                self.wait(ek, b.w)
            for ev in b.wm.values():
                self.wait(ek, ev)
        for b in mwrites:
            if b.w is not None:
                self.wait(ek, b.w)
            for ev in b.r.values():
                self.wait(ek, ev)
        for b in writes:
            for ev in b.wm.values():
                self.wait(ek, ev)
            for k2, pl in self.pend.items():
                if k2 == ek == "pe":
                    continue
                assert not any(pb is b for (pb, md) in pl), f"write to buffer with unsignaled access {b.name}"
            if b.w is not None:
                self.wait(ek, b.w)
            for ev in b.r.values():
                self.wait(ek, ev)

    @staticmethod
    def _reg(ev, reads, writes):
        for b in reads:
            old = b.r.get(ev[0])
            if old is None or old[2] < ev[2]:
                b.r[ev[0]] = ev
        for b in writes:
            b.w = ev
            b.r = {}
            b.wm = {}

    def op(self, ek, fn, reads=(), writes=(), signal=True):
        self._deps(ek, reads, writes)
        inst = fn(self.E[ek])
        if signal:
            self.ccnt[ek] += 1
            inst.then_inc(self.csem[ek], 1)
            ev = ("c_" + ek, self.csem[ek], self.ccnt[ek])
            for (pb, md) in self.pend[ek]:
                self._reg(ev, [pb] if md == "r" else [], [pb] if md == "w" else [])
            self.pend[ek] = []
            self._reg(ev, reads, writes)
            return ev
        for b in reads:
            self.pend[ek].append((b, "r"))
        for b in writes:
            self.pend[ek].append((b, "w"))
        return None

    def dma(self, q, fn, reads=(), writes=(), mwrites=()):
        self._deps(q, reads, writes, mwrites)
        j = self.dpos[q]
        self.dpos[q] = (j + 1) % self.ndma
        sem = self.dsem[q][j]
        key = f"d_{q}{j}"
        if self.dval[q][j] > 0:
            self.wait(q, (key, sem, self.dval[q][j]))
        inst = fn(self.E[q])
        self.dval[q][j] += 16
        inst.then_inc(sem, 16)
        ev = (key, sem, self.dval[q][j])
        self._reg(ev, reads, writes)
        for b in mwrites:
            old = b.wm.get(key)
            if old is None or old[2] < ev[2]:
                b.wm[key] = ev
        return ev


def _t5_bucket(rel):
    nb = 16
    max_exact = 8
    ret = (rel > 0).astype(np.int32) * nb
    n = np.abs(rel)
    large = max_exact + (np.log(np.maximum(n, 1) / max_exact) / np.log(128 / max_exact)
                         * (nb - max_exact)).astype(np.int32)
    large = np.minimum(large, nb - 1)
    return (ret + np.where(n < max_exact, n, large)).astype(np.int32)


QPERM = [0, 4, 1, 5, 2, 6, 3, 7]


def build(NSEQ=4, upto="C", dbg=False):
    nc = bass.Bass("TRN2", target_bir_lowering=False)
    NT = NSEQ * S
    NBLK = NSEQ * NB

    def din(name, shape, dt=F32):
        return nc.dram_tensor(name, list(shape), dt, kind="ExternalInput").ap()

    x_d = din("x", [NT, D])
    g1_d = din("norm_mix_g", [D])
    win_d = din("w_in", [D, IN_COLS])
    bg_d = din("b_gate", [2 * D])
    vg_d = din("vnorm_g", [512])
    wsp_d = din("w_spatial", [8, 128, 128])
    bsp_d = din("b_spatial", [8, 128])
    qg_d = din("q_norm_g", [64])
    kg_d = din("k_norm_g", [64])
    sink_d = din("attn_sink", [8])
    bias_d = din("bias_exp", [128, 3 * 8 * 128])
    mask_d = din("band_mask", [128, 3 * 8 * 128])
    wpa_d = din("w_proj_a", [512, D])
    wpb_d = din("w_proj_b", [512, D])
    wout_d = din("w_out", [D, D])
    g2_d = din("norm_ffn_g", [D])
    wr_d = din("w_router", [D, E])
    EDECL = E if upto == "C" else 1
    wge_d = din("w_gate_e", [EDECL, D, FF])
    wue_d = din("w_up_e", [EDECL, D, FF])
    wde_d = din("w_down_e", [EDECL, FF, D])
    ident_d = din("ident", [128, 128])
    offs_d = din("offs", [128, 1])
    flip_d = din("flip", [128, 128])

    out_d = nc.dram_tensor("out", [NT, D], F32, kind="ExternalOutput").ap()
    h2_d = nc.dram_tensor("h2d", [NT, D], BF16, kind="ExternalOutput" if dbg else "Internal").ap()
    if dbg:
        aff_dbg = nc.dram_tensor("aff_dbg", [128, S // 2], F32, kind="ExternalOutput").ap()
        idx_dbg = nc.dram_tensor("idx_dbg", [128, 2 * 64], U32, kind="ExternalOutput").ap()
        gat_dbg = nc.dram_tensor("gat_dbg", [128, 2 * 64], F32, kind="ExternalOutput").ap()

    es = contextlib.ExitStack()
    with es:
        dp = Dep(nc, es)
        op, dma = dp.op, dp.dma

        def sb(name, shape, dt):
            return es.enter_context(nc.sbuf_tensor(name, list(shape), dt))

        ps = es.enter_context(nc.psum_tensor("ps", [128, 8, 512], F32))
        psb = [Buf(f"psum{i}") for i in range(8)]
        ring = [0]

        def palloc(n=1):
            p = ring[0]
            if n == 2 and p % 2 == 1:
                p = (p + 1) % 8
            ring[0] = (p + n) % 8
            return p

        def pbf(b):
            return ps[:, b, :].bitcast(BF16)

        ident_f = sb("ident_f", [128, 128], F32)
        ident_b = sb("ident_b", [128, 128], BF16)
        neghalf = sb("neghalf", [128, 16], F32)
        aff_all = sb("aff_all", [128, S // 2], F32)
        flipm = sb("flipm", [128, 128], F32)
        B_ident = Buf("ident")
        B_aff_all = Buf("aff_all")
        B_const = Buf("const")

        dma("sp", lambda e: e.dma_start(out=ident_f[:], in_=ident_d[:, :]), writes=[B_ident])
        dma("sp", lambda e: e.dma_start(out=flipm[:], in_=flip_d[:, :]), writes=[B_ident])
        op("pool", lambda e: e.memset(aff_all[:], 0.0), writes=[B_aff_all])
        dma("pool", lambda e: e.dma_start(out=ident_b[:], in_=ident_d[:, :]), writes=[B_ident])
        op("pool", lambda e: e.memset(neghalf[:], -0.5), writes=[B_const])

        B_out = [Buf(f"out{s}") for s in range(NSEQ)]
        B_h2d = [Buf(f"h2d{s}") for s in range(NSEQ)]

        def rstd_from_ss(ss_ap, rstd_ap, n, inv_n, rb, wb, tmp_ap, tb):
            op("pool", lambda e: e.tensor_scalar(out=tmp_ap, in0=ss_ap, scalar1=inv_n, scalar2=EPS,
                                                 op0=ALU.mult, op1=ALU.add), reads=[rb], writes=[tb])
            op("pool", lambda e: e.tensor_tensor(out=rstd_ap, in0=tmp_ap, in1=neghalf[:, 0:n], op=ALU.pow),
               reads=[tb, B_const], writes=[wb])

        with contextlib.ExitStack() as esA:
            def sba(name, shape, dt):
                return esA.enter_context(nc.sbuf_tensor(name, list(shape), dt))

            W_in = sba("W_in", [128, 8, IN_COLS], BF16)
            Wpa = sba("Wpa", [128, 4, D], BF16)
            Wpb = sba("Wpb", [128, 4, D], BF16)
            Wout = sba("Wout", [128, 8, D], BF16)
            WsN = sba("WsN", [128, 8, 128], BF16)
            WsT = sba("WsT", [128, 8, 128], BF16)
            Wr = sba("Wr", [128, 8, E], F32)
            g1B = sba("g1B", [128, D], F32)
            g2B = sba("g2B", [128, D], F32)
            vgB = sba("vgB", [128, 512], F32)
            gqk = sba("gqk", [128, 128], F32)
            gkB = sba("gkB", [128, 64], F32)
            g2c = sba("g2c", [128, 8], F32)
            bsp = sba("bsp", [128, 8], F32)
            esink = sba("esink", [128, 8], F32)
            expb = sba("expb", [128, 3 * 8 * 128], BF16)
            B_W = Buf("paramsA")
            B_bg = Buf("bgrow")
            B_ones = Buf("onesr")
            B_Wing = [Buf(f"W_in_g{i}") for i in range(8)]
            B_Win = [Buf(f"W_in{i}") for i in range(8)]
            B_Wp = [Buf(f"Wp{i}") for i in range(8)]
            B_Wo = [Buf(f"Wout{i}") for i in range(8)]
            B_Ws, B_Wr, B_eb, B_bsp = Buf("Ws"), Buf("Wr"), Buf("expb"), Buf("bsp")
            bgrow = sba("bgrow", [128, 2 * D], BF16)
            onesr = sba("onesr", [128, 128], BF16)

            dma("pool", lambda e: e.dma_start(out=WsN[:], in_=wsp_d.rearrange("g p q -> p g q")), writes=[B_Ws])
            for kc in range(8):
                dma("pool", lambda e, kc=kc: e.dma_start(
                    out=W_in[:, kc, 0:1792], in_=win_d[kc * 128:(kc + 1) * 128, 0:1792]), writes=[B_Win[kc]])
            with nc.allow_non_contiguous_dma(reason="tiny one-time parameter loads"):
                dma("pool", lambda e: e.dma_start(out=bsp[:], in_=bsp_d.rearrange("g p -> p g")), writes=[B_bsp])
            for (t_, d_) in ((g1B[:], g1_d), (vgB[:], vg_d), (gqk[:, 0:64], qg_d), (gkB[:], kg_d), (esink[:], sink_d),
                             (g2B[:], g2_d)):
                dma("sp", lambda e, t_=t_, d_=d_: e.dma_start(out=t_, in_=d_.partition_broadcast(128)), mwrites=[B_W])
            op("dve", lambda e: e.memset(onesr[:], 1.0), writes=[B_ones])

            def load_gates():
                for kc in range(8):
                    dma("pool", lambda e, kc=kc: e.dma_start(
                        out=W_in[:, kc, 1792:IN_COLS], in_=win_d[kc * 128:(kc + 1) * 128, 1792:IN_COLS]),
                        writes=[B_Wing[kc]])
                op("pool", lambda e: e.memset(bgrow[:], 0.0), writes=[B_bg])
                dma("pool", lambda e: e.dma_start(out=bgrow[0:1, :], in_=bg_d.unsqueeze(0)), writes=[B_bg])

            def load_wp():
                for kc in range(4):
                    dma("pool", lambda e, kc=kc: e.dma_start(out=Wpa[:, kc, :], in_=wpa_d[kc * 128:(kc + 1) * 128, :]),
                        writes=[B_Wp[kc]])
                for kc in range(4):
                    dma("pool", lambda e, kc=kc: e.dma_start(out=Wpb[:, kc, :], in_=wpb_d[kc * 128:(kc + 1) * 128, :]),
                        writes=[B_Wp[4 + kc]])

            def load_wout_wr():
                for kc in range(8):
                    dma("pool", lambda e, kc=kc: e.dma_start(out=Wout[:, kc, :], in_=wout_d[kc * 128:(kc + 1) * 128, :]),
                        writes=[B_Wo[kc]])
                dma("pool", lambda e: e.dma_start(out=Wr[:], in_=wr_d.rearrange("(k p) e -> p k e", p=128)), writes=[B_Wr])
                with nc.allow_non_contiguous_dma(reason="tiny one-time parameter loads"):
                    dma("pool", lambda e: e.dma_start(out=g2c[:], in_=g2_d.rearrange("(k p) -> p k", p=128)),
                        writes=[B_Wr])

            op("dve", lambda e: e.scalar_tensor_tensor(out=gqk[:, 0:64], in0=gqk[:, 0:64], scalar=0.125, in1=gkB[:],
                                                       op0=ALU.mult, op1=ALU.mult), reads=[B_W], writes=[B_W])
            op("dve", lambda e: e.tensor_copy(out=gqk[:, 64:128], in_=gqk[:, 0:64]), reads=[B_W], writes=[B_W])
            op("act", lambda e: e.activation(out=esink[:], in_=esink[:], func=AF.Exp), reads=[B_W], writes=[B_W])
            t1 = sba("t1", [128, D], F32)
            B_t1 = Buf("t1")
            t2 = sba("t2", [128, D], F32)
            B_t2 = [Buf("t2a"), Buf("t2b")]
            pb_ = palloc()
            for g in range(8):
                op("pe", lambda e, g=g: e.transpose(out=pbf(pb_)[:, g * 128:(g + 1) * 128], in_=WsN[:, g, :],
                                                    identity=ident_b[:]),
                   reads=[B_Ws, B_ident], writes=[psb[pb_]], signal=(g == 7))
            op("act", lambda e: e.copy(out=WsT[:].rearrange("p g q -> p (g q)"), in_=pbf(pb_)[:, :]),
               reads=[psb[pb_]], writes=[B_Ws])

            xt = [sba(f"xt{i}", [128, D], F32) for i in range(4)]
            B_xt = [Buf(f"xt{i}") for i in range(4)]
            junk = [sba(f"junk{i}", [128, D if i != 1 else 512], BF16) for i in range(3)]
            B_junk = [Buf(f"junk{i}") for i in range(3)]
            st_ = sba("stats", [128, 64], F32)
            hb = [sba(f"hb{i}", [128, D], BF16) for i in range(2)]
            B_hb = [Buf(f"hb{i}") for i in range(2)]
            hT = [sba(f"hT{i}", [128, 8, 128], BF16) for i in range(2)]
            B_hT = [Buf(f"hT{i}") for i in range(2)]
            u = sba("u", [128, 512], BF16)
            B_u = Buf("u")
            va = sba("va", [128, 512], F32)
            B_va = Buf("va")
            vn = sba("vn", [128, 512], BF16)
            B_vn = Buf("vn")
            a_ = sba("a", [128, 512], BF16)
            B_a = [Buf(f"a{g}") for g in range(8)]
            aT = [sba(f"aT{i}", [128, 4, 128], BF16) for i in range(2)]
            B_aT = [Buf(f"aT{i}") for i in range(2)]
            sq = sba("sq", [128, 640], F32)
            B_sq = Buf("sq")
            qn = sba("qn", [128, 640], BF16)
            B_qn = Buf("qn")
            knt = sba("knt", [128, 128], F32)
            B_knt = Buf("knt")
            qT = [sba(f"qT{i}", [128, 4, 128], BF16) for i in range(2)]
            B_qT = [Buf(f"qT{i}") for i in range(2)]
            kT = sba("kT", [128, S], BF16)
            B_kT = [Buf(f"kT{i}") for i in range(NB)]
            vs = sba("vs", [128, NB, 2, 80], BF16)
            B_vs = [Buf(f"vs{i}") for i in range(NB)]
            gT = sba("gT", [128, 16, 128], BF16)
            B_gT = [Buf(f"gT{i}") for i in range(4)]
            et = [sba(f"et{i}", [128, 512], BF16) for i in range(6)]
            B_et = [Buf(f"et{i}") for i in range(6)]
            pT = [t[:].rearrange("p (c q) -> p c q", q=128) for t in et]
            B_pT = B_et
            o_ = sba("o", [128, 512], BF16)
            B_o = Buf("o")
            oT = sba("oT", [128, 4, 128], BF16)
            B_oT = Buf("oT")
            mT = sba("mT", [128, 8, 128], BF16)
            B_mT = [Buf("mTa"), Buf("mTb")]
            B_rstd2 = [Buf("rstd2a"), Buf("rstd2b")]
            h2t = sba("h2t", [128, D], BF16)
            B_h2t = Buf("h2t")
            x1T = sba("x1T", [128, 8, 128], F32)
            B_x1T = Buf("x1T")
            rt = sba("rt", [128, 64], F32)
            affb = [sba(f"affb{i}", [16, 128], F32) for i in range(2)]
            B_affb = [Buf(f"affb{i}") for i in range(2)]

            def stat(i, n=1):
                return st_[:, i:i + n]
            B_s = {k: Buf("st_" + k) for k in ("ss", "tmp", "rstd", "ssv", "tmpv", "rstdv", "ss10", "tmp10", "rstd10",
                                               "den", "rec", "ss2", "tmp2", "rstd2", "se", "rse")}
            SS, TMP, RSTD, SSV, TMPV, RSTDV = stat(0), stat(1), stat(2), stat(3), stat(4), stat(5)
            SS10, TMP10, RSTD10 = stat(8, 10), stat(18, 10), stat(28, 10)
            DEN, REC = stat(38, 8), stat(46, 8)
            SS2, TMP2, RSTD2, SE, RSE = stat(54), stat(55), stat(56), stat(57), stat(58)

            op("pool", lambda e: e.memset(vs[:].rearrange("p b k d -> p (b k d)"), 1.0), writes=B_vs)

            NBT = NSEQ * NB

            def XB(blk):
                return xt[blk % 4], B_xt[blk % 4]

            def ldx(blk):
                X, BX = XB(blk)
                r0 = blk * 128
                dma("sp", lambda e: e.dma_start(out=X[:], in_=x_d[r0:r0 + 128, :]), writes=[BX])

            def pre(blk):
                X, BX = XB(blk)
                hbi = blk % 2
                op("act", lambda e: e.activation(out=junk[0][:], in_=X[:], func=AF.Square, accum_out=SS),
                   reads=[BX], writes=[B_junk[0], B_s["ss"]])
                rstd_from_ss(SS, RSTD, 1, 1.0 / D, B_s["ss"], B_s["rstd"], TMP, B_s["tmp"])
                op("dve", lambda e: e.scalar_tensor_tensor(out=hb[hbi][:], in0=X[:], scalar=RSTD, in1=g1B[:],
                                                           op0=ALU.mult, op1=ALU.mult),
                   reads=[BX, B_s["rstd"], B_W], writes=[B_hb[hbi]])

            def Th(blk):
                par, hbi = blk % 2, blk % 2
                pb = palloc()
                for kc in range(8):
                    op("pe", lambda e, kc=kc: e.transpose(out=pbf(pb)[:, kc * 128:(kc + 1) * 128],
                                                          in_=hb[hbi][:, kc * 128:(kc + 1) * 128], identity=ident_b[:]),
                       reads=[B_hb[hbi], B_ident], writes=[psb[pb]], signal=(kc == 7))
                op("act", lambda e: e.copy(out=hT[par][:].rearrange("p k t -> p (k t)"), in_=pbf(pb)[:, :]),
                   reads=[psb[pb]], writes=[B_hT[par]])

            zbs = {}

            ZP = {"u": (0, 512), "va": (512, 512), "q": (1024, 512), "kv": (1536, 256)}

            def Zs(blk, parts):
                par = blk % 2
                zb = zbs.setdefault(blk, {})
                for nm in parts:
                    c0, w = ZP[nm]
                    b = palloc()
                    zb[nm] = b
                    for kc in range(8):
                        op("pe", lambda e, kc=kc, b=b, c0=c0, w=w: e.matmul(
                            ps[:, b, 0:w], lhsT=hT[par][:, kc, :], rhs=W_in[:, kc, c0:c0 + w],
                            start=(kc == 0), stop=(kc == 7)),
                           reads=[B_hT[par], B_Win[kc]], writes=[psb[b]], signal=(kc == 7))

            def S1e_uv(blk):
                zb = zbs[blk]
                op("act", lambda e: e.activation(out=u[:], in_=ps[:, zb["u"], :], func=AF.Gelu_apprx_tanh),
                   reads=[psb[zb["u"]]], writes=[B_u])
                op("act", lambda e: e.activation(out=va[:], in_=ps[:, zb["va"], :], func=AF.Gelu_apprx_tanh),
                   reads=[psb[zb["va"]]], writes=[B_va])
                op("act", lambda e: e.activation(out=junk[1][:], in_=va[:], func=AF.Square, accum_out=SSV),
                   reads=[B_va], writes=[B_junk[1], B_s["ssv"]])
                rstd_from_ss(SSV, RSTDV, 1, 1.0 / 512, B_s["ssv"], B_s["rstdv"], TMPV, B_s["tmpv"])
                op("dve", lambda e: e.scalar_tensor_tensor(out=vn[:], in0=va[:], scalar=RSTDV, in1=vgB[:],
                                                           op0=ALU.mult, op1=ALU.mult),
                   reads=[B_va, B_s["rstdv"], B_W], writes=[B_vn])

            B_sqk, B_qnk = Buf("sqk"), Buf("qnk")
            B_sk = {k: Buf("st_" + k) for k in ("ssk", "tmpk", "rstdk")}

            def S1e_q1(blk):
                zq = zbs[blk]["q"]
                op("act", lambda e: e.activation(out=sq[:, 0:512], in_=ps[:, zq, :], func=AF.Square),
                   reads=[psb[zq]], writes=[B_sq])
                op("dve", lambda e: e.reduce_sum(out=st_[:, 8:16], in_=sq[:, 0:512].rearrange("p (h d) -> p h d", d=64),
                                                 axis=AX.X), reads=[B_sq], writes=[B_s["ss10"]])
                rstd_from_ss(st_[:, 8:16], st_[:, 28:36], 8, 1.0 / 64, B_s["ss10"], B_s["rstd10"], st_[:, 18:26],
                             B_s["tmp10"])

            def S1e_q2(blk):
                zq = zbs[blk]["q"]
                op("dve", lambda e: e.tensor_tensor(
                    out=qn[:, 0:512].rearrange("p (h d) -> p h d", d=64),
                    in0=ps[:, zq, :].rearrange("p (h d) -> p h d", d=64),
                    in1=st_[:, 28:36].unsqueeze(2).broadcast_to([128, 8, 64]), op=ALU.mult),
                   reads=[psb[zq], B_s["rstd10"]], writes=[B_qn])

            def S1e_k(blk):
                n = blk % NB
                zk = zbs[blk]["kv"]
                op("act", lambda e: e.activation(out=sq[:, 512:640], in_=ps[:, zk, 0:128], func=AF.Square),
                   reads=[psb[zk]], writes=[B_sqk])
                op("act", lambda e: e.copy(out=vs[:, n, :, 0:64],
                                           in_=ps[:, zk, 128:256].rearrange("p (k d) -> p k d", d=64)),
                   reads=[psb[zk]], writes=[B_vs[n]])
                op("dve", lambda e: e.reduce_sum(out=st_[:, 16:18], in_=sq[:, 512:640].rearrange("p (h d) -> p h d", d=64),
                                                 axis=AX.X), reads=[B_sqk], writes=[B_sk["ssk"]])
                rstd_from_ss(st_[:, 16:18], st_[:, 36:38], 2, 1.0 / 64, B_sk["ssk"], B_sk["rstdk"], st_[:, 26:28],
                             B_sk["tmpk"])
                op("dve", lambda e: e.tensor_tensor(
                    out=knt[:].rearrange("p (h d) -> p h d", d=64),
                    in0=ps[:, zk, 0:128].rearrange("p (h d) -> p h d", d=64),
                    in1=st_[:, 36:38].unsqueeze(2).broadcast_to([128, 2, 64]), op=ALU.mult),
                   reads=[psb[zk], B_sk["rstdk"]], writes=[B_knt])
                op("dve", lambda e: e.tensor_tensor(out=qn[:, 512:640], in0=knt[:], in1=gqk[:], op=ALU.mult),
                   reads=[B_knt, B_W], writes=[B_qnk])

            def Tqk_SP(blk):
                n, par = blk % NB, blk % 2
                pb2 = palloc()
                for c in range(5):
                    op("pe", lambda e, c=c: e.transpose(out=pbf(pb2)[:, c * 128:(c + 1) * 128],
                                                        in_=qn[:, c * 128:(c + 1) * 128], identity=ident_b[:]),
                       reads=[B_qn, B_qnk, B_ident], writes=[psb[pb2]], signal=(c == 4))
                op("act", lambda e: e.copy(out=kT[:, n * 128:(n + 1) * 128], in_=pbf(pb2)[:, 512:640]),
                   reads=[psb[pb2]], writes=[B_kT[n]])
                op("act", lambda e: e.copy(out=qT[par][:].rearrange("p k t -> p (k t)"), in_=pbf(pb2)[:, 0:512]),
                   reads=[psb[pb2]], writes=[B_qT[par]])
                pb3 = palloc()
                for g in range(8):
                    op("pe", lambda e, g=g: e.matmul(ps[:, pb3, g * 64:(g + 1) * 64], lhsT=WsT[:, g, :],
                                                     rhs=vn[:, g * 64:(g + 1) * 64], start=True, stop=True),
                       reads=[B_vn, B_Ws], writes=[psb[pb3]], signal=(g == 7))
                for g in range(8):
                    op("dve", lambda e, g=g: e.scalar_tensor_tensor(
                        out=a_[:, g * 64:(g + 1) * 64], in0=ps[:, pb3, g * 64:(g + 1) * 64], scalar=bsp[:, g:g + 1],
                        in1=u[:, g * 64:(g + 1) * 64], op0=ALU.add, op1=ALU.mult),
                       reads=[psb[pb3], B_u, B_bsp], writes=[B_a[g]])

            def Ta(blk):
                par = blk % 2
                pb4 = palloc()
                for c in range(4):
                    op("pe", lambda e, c=c: e.transpose(out=pbf(pb4)[:, c * 128:(c + 1) * 128],
                                                        in_=a_[:, c * 128:(c + 1) * 128], identity=ident_b[:]),
                       reads=[B_a[2 * c], B_a[2 * c + 1], B_ident], writes=[psb[pb4]], signal=(c == 3))
                op("act", lambda e: e.copy(out=aT[par][:].rearrange("p k t -> p (k t)"), in_=pbf(pb4)[:, 0:512]),
                   reads=[psb[pb4]], writes=[B_aT[par]])

            def Gm(blk, q4s=(0, 1, 2, 3)):
                par = blk % 2
                for q4 in q4s:
                    b = palloc()
                    for j in range(4):
                        gc = q4 * 4 + j
                        op("pe", lambda e, b=b, j=j, gc=gc: e.matmul(
                            ps[:, b, j * 128:(j + 1) * 128], lhsT=bgrow[:, gc * 128:(gc + 1) * 128], rhs=onesr[:, :],
                            start=True, stop=False), reads=[B_ones, B_bg], writes=[psb[b]], signal=False)
                        for kc in range(8):
                            op("pe", lambda e, kc=kc, b=b, j=j, gc=gc: e.matmul(
                                ps[:, b, j * 128:(j + 1) * 128],
                                lhsT=W_in[:, kc, 1792 + gc * 128:1792 + (gc + 1) * 128], rhs=hT[par][:, kc, :],
                                start=False, stop=(kc == 7)),
                               reads=[B_hT[par], B_Wing[kc]], writes=[psb[b]], signal=(kc == 7 and j == 3))
                    op("act", lambda e, b=b, q4=q4: e.activation(
                        out=gT[:, q4 * 4:(q4 + 1) * 4, :].rearrange("p g t -> p (g t)"), in_=ps[:, b, :], func=AF.Tanh,
                        scale=0.5), reads=[psb[b]], writes=[B_gT[q4]])

            gTf = gT[:].rearrange("p g t -> p (g t)")

            def PAm(blk):
                par = blk % 2
                pab = palloc(2)
                for dc in range(8):
                    for kc in range(4):
                        op("pe", lambda e, dc=dc, kc=kc: e.matmul(
                            ps[:, pab + dc // 4, (dc % 4) * 128:(dc % 4 + 1) * 128],
                            lhsT=Wpa[:, kc, dc * 128:(dc + 1) * 128], rhs=aT[par][:, kc, :],
                            start=(kc == 0), stop=(kc == 3)),
                           reads=[B_aT[par], B_Wp[kc]], writes=[psb[pab + dc // 4]], signal=(kc == 3 and dc % 4 == 3))
                op("dve", lambda e: e.scalar_tensor_tensor(
                    out=t1[:], in0=gTf[:, 0:1024], scalar=1.0, in1=ps[:, pab:pab + 2, :].rearrange("p k c -> p (k c)"),
                    op0=ALU.add, op1=ALU.mult), reads=[B_gT[0], B_gT[1], psb[pab], psb[pab + 1]], writes=[B_t1])

            def SCm(blk):
                m, par = blk % NB, blk % 2
                kbs = [kb for kb in range(3) if 0 <= m + kb - 1 < NB]
                for kv in range(2):
                    for kb in kbs:
                        kblk = m + kb - 1
                        b = palloc()
                        ei = kv * 3 + kb
                        op("pe", lambda e, b=b, kv=kv, kblk=kblk: e.matmul(
                            ps[:, b, :], lhsT=kT[kv * 64:(kv + 1) * 64, kblk * 128:(kblk + 1) * 128],
                            rhs=qT[par][kv * 64:(kv + 1) * 64, :, :], start=True, stop=True),
                           reads=[B_kT[kblk], B_qT[par]], writes=[psb[b]], signal=True)
                        op("act", lambda e, b=b, ei=ei: e.activation(out=et[ei][:], in_=ps[:, b, :], func=AF.Exp),
                           reads=[psb[b]], writes=[B_et[ei]])
                        op("dve", lambda e, kb=kb, kv=kv, ei=ei: e.tensor_tensor(
                            out=et[ei][:], in0=et[ei][:],
                            in1=expb[:, (kb * 8 + kv * 4) * 128:(kb * 8 + kv * 4 + 4) * 128], op=ALU.mult),
                           reads=[B_eb], writes=[B_et[ei]])

            def PVm(blk):
                m = blk % NB
                kbs = [kb for kb in range(3) if 0 <= m + kb - 1 < NB]
                ob = palloc(2)
                for kv in range(2):
                    for c in range(4):
                        for i, kb in enumerate(kbs):
                            kblk = m + kb - 1
                            ei = kv * 3 + kb
                            op("pe", lambda e, kv=kv, c=c, ei=ei, kblk=kblk, i=i: e.matmul(
                                ps[:, ob + kv, c * 65:(c + 1) * 65], lhsT=pT[ei][:, c, :], rhs=vs[:, kblk, kv, 0:65],
                                start=(i == 0), stop=(i == len(kbs) - 1)),
                               reads=[B_pT[ei], B_vs[kblk]], writes=[psb[ob + kv]],
                               signal=(c == 3 and i == len(kbs) - 1))
                ov = ps[:, ob:ob + 2, 0:260].rearrange("p k (c d) -> p k c d", d=65)
                op("dve", lambda e: e.tensor_tensor(out=DEN.rearrange("p (k c) -> p k c", c=4), in0=ov[:, :, :, 64],
                                                    in1=esink[:].rearrange("p (k c) -> p k c", c=4), op=ALU.add),
                   reads=[psb[ob], psb[ob + 1], B_W], writes=[B_s["den"]])
                op("dve", lambda e: e.reciprocal(out=REC, in_=DEN), reads=[B_s["den"]], writes=[B_s["rec"]])
                op("dve", lambda e: e.tensor_tensor(
                    out=o_[:].rearrange("p (k c d) -> p k c d", c=4, d=64), in0=ov[:, :, :, 0:64],
                    in1=st_[:, 46:54].rearrange("p (k c) -> p k c", c=4).unsqueeze(3).broadcast_to([128, 2, 4, 64]),
                    op=ALU.mult), reads=[psb[ob], psb[ob + 1], B_s["rec"]], writes=[B_o])

            def Tom(blk):
                pb = palloc()
                for c in range(4):
                    op("pe", lambda e, c=c: e.transpose(out=pbf(pb)[:, c * 128:(c + 1) * 128],
                                                        in_=o_[:, c * 128:(c + 1) * 128], identity=ident_b[:]),
                       reads=[B_o, B_ident], writes=[psb[pb]], signal=(c == 3))
                op("act", lambda e: e.copy(out=oT[:].rearrange("p k t -> p (k t)"), in_=pbf(pb)[:, 0:512]),
                   reads=[psb[pb]], writes=[B_oT])

            def PB_Y(blk):
                X, BX = XB(blk)
                r0 = blk * 128
                pbb = palloc(2)
                for hf in range(2):
                    for dc in range(hf * 4, hf * 4 + 4):
                        for kc in range(4):
                            op("pe", lambda e, dc=dc, kc=kc: e.matmul(
                                ps[:, pbb + dc // 4, (dc % 4) * 128:(dc % 4 + 1) * 128],
                                lhsT=Wpb[:, kc, dc * 128:(dc + 1) * 128], rhs=oT[:, kc, :],
                                start=(kc == 0), stop=(kc == 3)),
                               reads=[B_oT, B_Wp[4 + kc]], writes=[psb[pbb + hf]], signal=(kc == 3 and dc % 4 == 3))
                    op("dve", lambda e, hf=hf: e.scalar_tensor_tensor(
                        out=t2[:, hf * 512:(hf + 1) * 512], in0=gTf[:, 1024 + hf * 512:1024 + (hf + 1) * 512], scalar=1.0,
                        in1=ps[:, pbb + hf, :], op0=ALU.add, op1=ALU.mult),
                       reads=[B_gT[2 + hf], psb[pbb + hf]], writes=[B_t2[hf]])
                    op("dve", lambda e, hf=hf: e.tensor_tensor(
                        out=mT[:].rearrange("p k t -> p (k t)")[:, hf * 512:(hf + 1) * 512],
                        in0=t1[:, hf * 512:(hf + 1) * 512], in1=t2[:, hf * 512:(hf + 1) * 512], op=ALU.add),
                       reads=[B_t1, B_t2[hf]], writes=[B_mT[hf]])

            def Ym(blk):
                X, BX = XB(blk)
                r0 = blk * 128
                yb = palloc(2)
                for hf in range(2):
                    for ch in range(2):
                        for kc in range(hf * 4, hf * 4 + 4):
                            op("pe", lambda e, ch=ch, kc=kc: e.matmul(
                                ps[:, yb + ch, :], lhsT=mT[:, kc, :], rhs=Wout[:, kc, ch * 512:(ch + 1) * 512],
                                start=(kc == 0), stop=(kc == 7), skip_group_check=True),
                               reads=[B_mT[hf], B_Wo[kc]], writes=[psb[yb + ch]], signal=(kc == 7))
                op("dve", lambda e: e.scalar_tensor_tensor(
                    out=X[:], in0=ps[:, yb:yb + 2, :].rearrange("p k c -> p (k c)"), scalar=0.5, in1=X[:],
                    op0=ALU.mult, op1=ALU.add), reads=[psb[yb], psb[yb + 1], BX], writes=[BX])
                dma("sp", lambda e: e.dma_start(out=out_d[r0:r0 + 128, :], in_=X[:]), reads=[BX], mwrites=[B_out[blk // NB]])
                op("act", lambda e: e.activation(out=junk[2][:], in_=X[:], func=AF.Square, accum_out=SS2),
                   reads=[BX], writes=[B_junk[2], B_s["ss2"]])
                rstd_from_ss(SS2, stat(59 + blk % 2), 1, 1.0 / D, B_s["ss2"], B_rstd2[blk % 2], TMP2, B_s["tmp2"])
                op("dve", lambda e: e.scalar_tensor_tensor(out=h2t[:], in0=X[:], scalar=stat(59 + blk % 2), in1=g2B[:],
                                                           op0=ALU.mult, op1=ALU.mult),
                   reads=[BX, B_rstd2[blk % 2], B_W], writes=[B_h2t])
                dma("sp", lambda e: e.dma_start(out=h2_d[r0:r0 + 128, :], in_=h2t[:]), reads=[B_h2t],
                    mwrites=[B_h2d[blk // NB]])

            def Rt_a(blk):
                X, BX = XB(blk)
                s, m = divmod(blk, NB)
                R2 = stat(59 + blk % 2)
                xb = palloc(2)
                for kc in range(8):
                    op("pe", lambda e, kc=kc: e.transpose(out=ps[:, xb + kc // 4, (kc % 4) * 128:(kc % 4 + 1) * 128],
                                                          in_=X[:, kc * 128:(kc + 1) * 128], identity=ident_f[:]),
                       reads=[BX, B_ident], writes=[psb[xb + kc // 4]], signal=(kc % 4 == 3))
                op("act", lambda e: e.copy(out=x1T[:].rearrange("p k t -> p (k t)"),
                                           in_=ps[:, xb:xb + 2, :].rearrange("p k c -> p (k c)")),
                   reads=[psb[xb], psb[xb + 1]], writes=[B_x1T])

            lbs = {}

            def Rt_b(blk):
                R2 = stat(59 + blk % 2)
                lb = palloc()
                lbs[blk] = lb
                for kc in range(8):
                    op("pe", lambda e, kc=kc: e.matmul(ps[:, lb, 0:E], lhsT=x1T[:, kc, :], rhs=Wr[:, kc, :],
                                                       start=(kc == 0), stop=(kc == 7)),
                       reads=[B_x1T, B_Wr], writes=[psb[lb]], signal=(kc == 7))
                B_rt = B_s["se"]
                op("act", lambda e: e.activation(out=rt[:, 0:E], in_=ps[:, lb, 0:E], func=AF.Exp, scale=R2,
                                                 accum_out=SE),
                   reads=[psb[lb], B_rstd2[blk % 2]], writes=[B_rt])
                op("dve", lambda e: e.reciprocal(out=RSE, in_=SE), reads=[B_rt], writes=[B_s["rse"]])
                op("dve", lambda e: e.tensor_scalar(out=rt[:, 16:32], in0=rt[:, 0:E], scalar1=RSE, scalar2=None,
                                                    op0=ALU.mult), reads=[B_rt, B_s["rse"]], writes=[B_s["rse"]])

            def Rt_c(blk):
                s, m = divmod(blk, NB)
                ab = palloc()
                op("pe", lambda e: e.transpose(out=ps[0:E, ab, 0:128], in_=rt[:, 16:32], identity=ident_f[:]),
                   reads=[B_s["rse"], B_ident], writes=[psb[ab]], signal=True)
                af = affb[blk % 2]
                op("act", lambda e: e.copy(out=af[:], in_=ps[0:E, ab, 0:128]), reads=[psb[ab]], writes=[B_affb[blk % 2]])
                hf_, mm_ = divmod(m, NB // 2)
                dma("sp", lambda e: e.dma_start(out=aff_all[hf_ * 64 + s * E:hf_ * 64 + (s + 1) * E,
                                                            mm_ * 128:(mm_ + 1) * 128], in_=af[:]),
                    reads=[B_affb[blk % 2]], mwrites=[B_aff_all])

            ldx(0)
            if NBT > 1:
                ldx(1)
            pre(0)
            Th(0)
            Zs(0, ("q", "kv"))
            S1e_q1(0)
            S1e_q2(0)
            S1e_k(0)
            for c3 in range(3):
                dma("sp", lambda e, c3=c3: e.dma_start(out=t1[:], in_=bias_d[:, c3 * 1024:(c3 + 1) * 1024]), writes=[B_t1])
                dma("sp", lambda e, c3=c3: e.dma_start(out=t2[:], in_=mask_d[:, c3 * 1024:(c3 + 1) * 1024]), writes=B_t2)
                op("act", lambda e: e.activation(out=t1[:], in_=t1[:], func=AF.Exp), reads=[B_t1], writes=[B_t1])
                op("dve", lambda e, c3=c3: e.tensor_tensor(out=expb[:, c3 * 1024:(c3 + 1) * 1024], in0=t1[:], in1=t2[:],
                                                           op=ALU.mult), reads=[B_t1] + B_t2, writes=[B_eb])
            for i in range(NBT + 2):
                n, m, r = i, i - 1, i - 2
                hn, hm, hr, hn1 = n < NBT, 0 <= m < NBT, 0 <= r < NBT, n + 1 < NBT
                if hn:
                    Zs(n, ("u", "va"))
                    S1e_uv(n)
                if hn1:
                    pre(n + 1)
                if i == 0:
                    load_gates()
                if hm:
                    Gm(m, (0, 1))
                if hr:
                    Rt_a(r)
                if n + 2 < NBT:
                    ldx(n + 2)
                if hm:
                    PAm(m)
                if hn:
                    Tqk_SP(n)
                    zbs.pop(n)
                if i == 0:
                    load_wp()
                if hr:
                    Rt_b(r)
                if hm:
                    SCm(m)
                    Gm(m, (2,))
                    PVm(m)
                    Gm(m, (3,))
                if hn1:
                    Th(n + 1)
                if hr:
                    Rt_c(r)
                if hm:
                    Tom(m)
                if hn1:
                    Zs(n + 1, ("q",))
                    S1e_q1(n + 1)
                if hm:
                    PB_Y(m)
                if hn1:
                    S1e_q2(n + 1)
                    Zs(n + 1, ("kv",))
                    S1e_k(n + 1)
                if i == 0:
                    load_wout_wr()
                if hn:
                    Ta(n)
                if hm:
                    Ym(m)
                if i == min(1, NBT - 1):
                    for kc in range(8):
                        op("dve", lambda e, kc=kc: e.tensor_scalar(out=Wr[:, kc, :], in0=Wr[:, kc, :], scalar1=g2c[:, kc:kc + 1],
                                                                   scalar2=None, op0=ALU.mult), reads=[B_Wr], writes=[B_Wr])

            fin = Buf("finA")
            allb = ([B_W, B_ones, B_bg, B_Ws, B_Wr, B_eb, B_bsp, B_u, B_va, B_vn, B_sq, B_sqk, B_qn, B_qnk, B_knt, B_o, B_oT, B_t1,
                     B_h2t, B_x1T] + B_a + B_Wing + B_Win + B_Wp + B_Wo + B_junk + B_hb + B_gT + B_t2 + B_mT + B_rstd2 + B_xt + B_hT + B_aT + B_qT + B_kT
                    + B_vs + B_et + B_affb + list(B_s.values()) + list(B_sk.values()) + psb)
            op("dve", lambda e: e.memset(rt[:, 32:40], 0.0), reads=[], writes=allb + [fin])

        NSE = NSEQ * E
        for ek in ("pe", "act", "pool", "sp", "dve"):
            dp._deps(ek, [fin], [])
        if dbg:
            dma("sp", lambda e: e.dma_start(out=aff_dbg[:, :], in_=aff_all[:, :]), reads=[B_aff_all, fin])
        NR = 3
        if upto == "C":
            WgQ = [sb(f"WgQ{i}", [128, 8, 512], BF16) for i in range(NR)]
            WuQ = [sb(f"WuQ{i}", [128, 8, 512], BF16) for i in range(NR)]
            WdQ = [sb(f"WdQ{i}", [128, 4, D], BF16) for i in range(NR)]
            B_WgQ = [Buf(f"WgQ{i}") for i in range(NR)]
            B_WuQ = [Buf(f"WuQ{i}") for i in range(NR)]
            B_WdQ = [Buf(f"WdQ{i}") for i in range(NR)]
            xg = [sb(f"xg{i}", [128, D], BF16) for i in range(2 * NSEQ)]
            B_xg = [Buf(f"xg{i}") for i in range(2 * NSEQ)]
            xgT = [sb(f"xgT{i}", [128, 8, NSEQ * CAP], BF16) for i in range(2)]
            B_xgT = [[Buf(f"xgT{i}_{s}") for s in range(NSEQ)] for i in range(2)]
            yacc = [sb(f"yacc{s}", [128, 2, D], F32) for s in range(NSEQ)]
            B_yacc = [Buf(f"yacc{s}") for s in range(NSEQ)]
            sg = [sb(f"sg{i}", [128, 512], F32) for i in range(2)]
            B_sg = [Buf(f"sg{i}") for i in range(2)]
            hid = [sb(f"hid{i}", [128, 512], BF16) for i in range(8)]
            B_hid = [Buf(f"hid{i}") for i in range(8)]
            rgc = [0]

            def palloc_c():
                rgc[0] = (rgc[0] + 1) % 4
                return 4 + rgc[0]

            def load_chunk(k):
                e_, Q = divmod(k, 4)
                sl = k % NR
                c0 = Q * 512
                dma("pool", lambda e: e.dma_start(
                    out=WgQ[sl][:], in_=wge_d[e_, :, c0:c0 + 512].rearrange("(k p) c -> p k c", p=128)),
                    writes=[B_WgQ[sl]])
                dma("pool", lambda e: e.dma_start(
                    out=WuQ[sl][:], in_=wue_d[e_, :, c0:c0 + 512].rearrange("(k p) c -> p k c", p=128)),
                    writes=[B_WuQ[sl]])
                dma("pool", lambda e: e.dma_start(
                    out=WdQ[sl][:], in_=wde_d[e_, c0:c0 + 512, :].rearrange("(k p) c -> p k c", p=128)),
                    writes=[B_WdQ[sl]])

            def prefetch_c():
                load_chunk(0)
                load_chunk(1)
                load_chunk(2)

            def gathers(e_):
                for s in range(NSEQ):
                    se = s * E + e_
                    for h in range(2):
                        xi = s * 2 + h
                        dma("pool", lambda e, h=h, xi=xi, se=se: e.indirect_dma_start(
                            out=xg[xi][:], out_offset=None, in_=h2_d[:, :],
                            in_offset=bass.IndirectOffsetOnAxis(ap=idxT[:, h * 64 + se:h * 64 + se + 1], axis=0)),
                            reads=[B_idxT] + B_h2d, writes=[B_xg[xi]])

            def xg_transposes(e_, s):
                par = e_ % 2
                for h in range(2):
                    xi = s * 2 + h
                    tb = palloc_c()
                    for kc in range(8):
                        op("pe", lambda e, kc=kc, xi=xi, tb=tb: e.transpose(
                            out=pbf(tb)[:, kc * 128:(kc + 1) * 128], in_=xg[xi][:, kc * 128:(kc + 1) * 128],
                            identity=ident_b[:]), reads=[B_xg[xi], B_ident], writes=[psb[tb]], signal=(kc == 7))
                    op("act", lambda e, h=h, tb=tb: e.copy(
                        out=xgT[par][:, :, s * CAP + h * 128:s * CAP + (h + 1) * 128],
                        in_=pbf(tb)[:, :].rearrange("p (k t) -> p k t", t=128)),
                       reads=[psb[tb]], writes=[B_xgT[par][s]])

        idxT = sb("idxT", [128, 2 * 64], U32)
        gatT = sb("gatT", [128, 2 * 64], F32)
        B_idxT = Buf("idxT")
        B_gatT = Buf("gatT")
        if upto in ("B", "C"):
            op("dve", lambda e: e.memset(idxT[:], 0), reads=[fin], writes=[B_idxT])
            op("dve", lambda e: e.memset(gatT[:], 0.0), reads=[fin], writes=[B_gatT])
            if upto == "C":
                prefetch_c()
            if True:
                HS = S // 2
                vals = sb("vals", [128, CAP], F32)
                idxu = sb("idxu", [128, CAP], U32)
                idxf = sb("idxf", [128, CAP], F32)
                work = sb("work", [128, HS], F32)
                offs = sb("offs_sb", [128, 1], F32)
                vT = sb("vT_sb", [128, 2, 128], F32)
                iT = sb("iT_sb", [128, 2, 128], F32)
                mrg = sb("mrg", [128, 3, 128], F32)
                B_vals, B_idxu, B_idxf, B_work, B_offs = Buf("vals"), Buf("idxu"), Buf("idxf"), Buf("work"), Buf("offs")
                B_vT, B_iT, B_mrg = Buf("vT"), Buf("iT"), Buf("mrg")
                dma("sp", lambda e: e.dma_start(out=offs[:], in_=offs_d[:, :]), writes=[B_offs])
                cur, Bcur = aff_all, B_aff_all
                for r in range(CAP // 8):
                    op("dve", lambda e, r=r, cur=cur: e.max(out=vals[:, r * 8:(r + 1) * 8], in_=cur[:, :]),
                       reads=[Bcur, fin], writes=[B_vals])
                    op("dve", lambda e, r=r, cur=cur: e.max_index(out=idxu[:, r * 8:(r + 1) * 8],
                                                                  in_max=vals[:, r * 8:(r + 1) * 8],
                                                                  in_values=cur[:, :]),
                       reads=[Bcur, B_vals], writes=[B_idxu])
                    if r < CAP // 8 - 1:
                        op("dve", lambda e, r=r, cur=cur: e.match_replace(
                            out=work[:, :], in_to_replace=vals[:, r * 8:(r + 1) * 8], in_values=cur[:, :],
                            imm_value=-1.0), reads=[Bcur, B_vals], writes=[B_work])
                        cur, Bcur = work, B_work
                op("dve", lambda e: e.tensor_copy(out=idxf[:, :], in_=idxu[:, :]), reads=[B_idxu], writes=[B_idxf])
                op("dve", lambda e: e.tensor_scalar(out=idxf[:, :], in0=idxf[:, :], scalar1=offs[:, 0:1],
                                                    scalar2=None, op0=ALU.add), reads=[B_idxf, B_offs], writes=[B_idxf])
                tb, tb2 = palloc(), palloc()
                for h in range(2):
                    op("pe", lambda e, h=h: e.transpose(out=ps[:, tb, h * 128:(h + 1) * 128],
                                                        in_=vals[:, h * 128:(h + 1) * 128], identity=ident_f[:]),
                       reads=[B_vals, B_ident], writes=[psb[tb]], signal=(h == 1))
                for h in range(2):
                    op("pe", lambda e, h=h: e.transpose(out=ps[:, tb2, h * 128:(h + 1) * 128],
                                                        in_=idxf[:, h * 128:(h + 1) * 128], identity=ident_f[:]),
                       reads=[B_idxf, B_ident], writes=[psb[tb2]], signal=(h == 1))
                op("dve", lambda e: e.tensor_copy(out=vT[:].rearrange("p h r -> p (h r)"), in_=ps[:, tb, 0:256]),
                   reads=[psb[tb]], writes=[B_vT])
                op("dve", lambda e: e.tensor_copy(out=iT[:].rearrange("p h r -> p (h r)"), in_=ps[:, tb2, 0:256]),
                   reads=[psb[tb2]], writes=[B_iT])
                fb, fb2 = palloc(), palloc()
                for h in range(2):
                    op("pe", lambda e, h=h: e.matmul(ps[:, fb, h * 64:(h + 1) * 64], lhsT=flipm[:],
                                                     rhs=vT[:, 1 - h, 64:128], start=True, stop=True),
                       reads=[B_vT, B_ident], writes=[psb[fb]], signal=(h == 1))
                for h in range(2):
                    op("pe", lambda e, h=h: e.matmul(ps[:, fb2, h * 64:(h + 1) * 64], lhsT=flipm[:],
                                                     rhs=iT[:, 1 - h, 64:128], start=True, stop=True),
                       reads=[B_iT, B_ident], writes=[psb[fb2]], signal=(h == 1))
                A3 = vT[:, :, 0:64]
                I3 = iT[:, :, 0:64]
                Br3 = ps[:, fb, 0:128].rearrange("p (h r) -> p h r", r=64)
                Ir3 = ps[:, fb2, 0:128].rearrange("p (h r) -> p h r", r=64)
                g3 = gatT[:].rearrange("p (h r) -> p h r", r=64)
                op("dve", lambda e: e.tensor_tensor(out=g3, in0=A3, in1=Br3, op=ALU.max),
                   reads=[B_vT, psb[fb]], writes=[B_gatT])
                op("dve", lambda e: e.tensor_tensor(out=mrg[:, 0, :].rearrange("p (h r) -> p h r", r=64), in0=A3, in1=Br3,
                                                    op=ALU.is_ge), reads=[B_vT, psb[fb]], writes=[B_mrg])
                op("dve", lambda e: e.tensor_tensor(out=mrg[:, 1, :].rearrange("p (h r) -> p h r", r=64), in0=I3, in1=Ir3,
                                                    op=ALU.subtract), reads=[B_iT, psb[fb2], B_mrg], writes=[B_mrg])
                op("dve", lambda e: e.tensor_tensor(out=mrg[:, 2, :], in0=mrg[:, 1, :], in1=mrg[:, 0, :], op=ALU.mult),
                   reads=[B_mrg], writes=[B_mrg])
                op("dve", lambda e: e.tensor_tensor(out=mrg[:, 1, :], in0=mrg[:, 2, :], in1=ps[:, fb2, 0:128], op=ALU.add),
                   reads=[B_mrg, psb[fb2]], writes=[B_mrg])
                op("dve", lambda e: e.tensor_copy(out=idxT[:], in_=mrg[:, 1, :]), reads=[B_mrg], writes=[B_idxT])
                finB = Buf("finB")
                op("dve", lambda e: e.memset(offs[:], 0.0),
                   reads=[B_idxT, B_gatT], writes=[B_vals, B_idxu, B_idxf, B_work, B_offs, B_vT, B_iT, B_mrg,
                                                   psb[tb], psb[tb2], psb[fb], psb[fb2], finB])
            if dbg:
                dma("sp", lambda e: e.dma_start(out=idx_dbg[:, :], in_=idxT[:]), reads=[B_idxT, finB])
                dma("sp", lambda e: e.dma_start(out=gat_dbg[:, :], in_=gatT[:]), reads=[B_gatT, finB])

        if upto == "C":
            NP = NSEQ // 2
            NCH = E * 4
            units = [(k, sp) for k in range(NCH) for sp in range(NP)]

            def GU2(k, sp, up_, js):
                e_, Q = divmod(k, 4)
                sl, par = k % NR, e_ % 2
                for j in js:
                    ga, gu_ = palloc_c(), palloc_c()
                    rhs = lambda kc: xgT[par][:, kc, sp * 512:(sp + 1) * 512]
                    for kc in range(8):
                        op("pe", lambda e, kc=kc: e.matmul(
                            ps[:, ga, :], lhsT=WgQ[sl][:, kc, j * 128:(j + 1) * 128], rhs=rhs(kc),
                            start=(kc == 0), stop=(kc == 7)),
                           reads=[B_WgQ[sl], B_xgT[par][2 * sp], B_xgT[par][2 * sp + 1]], writes=[psb[ga]], signal=(kc == 7))
                    for kc in range(8):
                        op("pe", lambda e, kc=kc: e.matmul(
                            ps[:, gu_, :], lhsT=WuQ[sl][:, kc, j * 128:(j + 1) * 128], rhs=rhs(kc),
                            start=(kc == 0), stop=(kc == 7)),
                           reads=[B_WuQ[sl], B_xgT[par][2 * sp], B_xgT[par][2 * sp + 1]], writes=[psb[gu_]], signal=(kc == 7))
                    sgi = j % 2
                    hi = up_ * 4 + j
                    op("act", lambda e, sgi=sgi: e.activation(out=sg[sgi][:], in_=ps[:, ga, :], func=AF.Silu),
                       reads=[psb[ga]], writes=[B_sg[sgi]])
                    op("dve", lambda e, sgi=sgi, hi=hi: e.tensor_tensor(
                        out=hid[hi][:], in0=sg[sgi][:], in1=ps[:, gu_, :], op=ALU.mult),
                       reads=[B_sg[sgi], psb[gu_]], writes=[B_hid[hi]])

            def DN(k, sp, up_, sl_):
                e_, Q = divmod(k, 4)
                sl = k % NR
                s = 2 * sp + sl_
                se = s * E + e_
                for st in range(2):
                    for dh in range(2):
                        for j in range(4):
                            hi = up_ * 4 + j
                            op("pe", lambda e, st=st, dh=dh, j=j, hi=hi: e.matmul(
                                ps[:, st * 2 + dh, :], lhsT=hid[hi][:, sl_ * 256 + st * 128:sl_ * 256 + (st + 1) * 128],
                                rhs=WdQ[sl][:, j, dh * 512:(dh + 1) * 512], start=(j == 0), stop=(j == 3)),
                               reads=[B_hid[hi], B_WdQ[sl]], writes=[psb[st * 2 + dh]], signal=(j == 3 and dh == 1))
                for st in range(2):
                    b0 = st * 2
                    src = ps[:, b0:b0 + 2, :].rearrange("p k c -> p (k c)")
                    gcol = gatT[:, st * 64 + se:st * 64 + se + 1]
                    if Q == 0:
                        op("act", lambda e, st=st, src=src, gcol=gcol: e.activation(
                            out=yacc[s][:, st, :], in_=src, func=AF.Copy, scale=gcol),
                           reads=[psb[b0], psb[b0 + 1], B_gatT], writes=[B_yacc[s]])
                    else:
                        op("dve", lambda e, st=st, src=src, gcol=gcol: e.scalar_tensor_tensor(
                            out=yacc[s][:, st, :], in0=src, scalar=gcol, in1=yacc[s][:, st, :],
                            op0=ALU.mult, op1=ALU.add),
                           reads=[psb[b0], psb[b0 + 1], B_gatT, B_yacc[s]], writes=[B_yacc[s]])
                if Q == 3:
                    for st in range(2):
                        dma("pool", lambda e, st=st: e.indirect_dma_start(
                            out=out_d[:, :], out_offset=bass.IndirectOffsetOnAxis(
                                ap=idxT[:, st * 64 + se:st * 64 + se + 1], axis=0),
                            in_=yacc[s][:, st, :], in_offset=None, compute_op=ALU.add),
                            reads=[B_yacc[s], B_idxT], writes=[B_out[s]])

            gathers(0)
            for s in range(NSEQ):
                xg_transposes(0, s)
            for ui, (k, sp) in enumerate(units):
                e_, Q = divmod(k, 4)
                GU2(k, sp, ui % 2, (0, 1))
                if ui >= 1:
                    pk, psp = units[ui - 1]
                    DN(pk, psp, (ui - 1) % 2, 0)
                GU2(k, sp, ui % 2, (2, 3))
                if ui >= 1:
                    DN(pk, psp, (ui - 1) % 2, 1)
                if sp == 0 and k >= 1 and k + 2 < NCH:
                    load_chunk(k + 2)
                if Q == 1 and sp == 0 and e_ + 1 < E:
                    gathers(e_ + 1)
                if Q == 3 and e_ + 1 < E:
                    xg_transposes(e_ + 1, 2 * sp)
                    xg_transposes(e_ + 1, 2 * sp + 1)
            lk, lsp = units[-1]
            DN(lk, lsp, (len(units) - 1) % 2, 0)
            DN(lk, lsp, (len(units) - 1) % 2, 1)
            finC = Buf("finC")
            allc = (B_WgQ + B_WuQ + B_WdQ + B_xg + B_xgT[0] + B_xgT[1] + B_yacc + B_sg + B_hid + psb)
            op("dve", lambda e: e.memset(neghalf[:, 0:1], 0.0), reads=[], writes=allc + [B_const, finC])

        tail = B_out + B_h2d + ([B_aff_all, B_idxT, B_gatT] if dbg else [])
        dp._deps("sp", tail, tail)
        dp._deps("pool", tail, tail)
    return nc


def _host_tables(rel_bias_table):
    j = np.arange(128)[:, None, None]
    kb = np.arange(3)[None, :, None]
    q = np.arange(128)[None, None, :]
    rel = (kb - 1) * 128 + j - q
    bucket = _t5_bucket(rel)
    bias = rel_bias_table[bucket]
    bias = np.ascontiguousarray(bias.transpose(0, 1, 3, 2)).reshape(128, 3 * 8 * 128).astype(np.float32)
    mask = (np.abs(rel) <= 128).astype(np.float32)
    mask = np.ascontiguousarray(np.broadcast_to(mask[:, :, None, :], (128, 3, 8, 128))).reshape(128, 3 * 8 * 128)
    return bias, mask


def make_in_maps(inputs, ncores, nseq, edecl=E):
    f = lambda k: np.ascontiguousarray(np.asarray(inputs[k], dtype=np.float32))
    x = f("x")
    w_in = f("w_in")[0]
    qcols = np.concatenate([np.arange(1024 + h * 64, 1024 + (h + 1) * 64) for h in QPERM])
    cols = np.concatenate([np.arange(0, 1024), qcols, np.arange(1536, IN_COLS)])
    w_in_p = np.ascontiguousarray(w_in[:, cols])
    bias, mask = _host_tables(f("rel_bias_table"))
    common = {
        "norm_mix_g": f("norm_mix_g")[0], "w_in": w_in_p, "b_gate": f("b_gate")[0], "vnorm_g": f("vnorm_g")[0],
        "w_spatial": f("w_spatial")[0], "b_spatial": f("b_spatial")[0], "q_norm_g": f("q_norm_g")[0],
        "k_norm_g": f("k_norm_g")[0], "attn_sink": f("attn_sink")[0], "bias_exp": bias, "band_mask": mask,
        "w_proj_a": f("w_proj_a")[0], "w_proj_b": f("w_proj_b")[0], "w_out": f("w_out")[0],
        "norm_ffn_g": f("norm_ffn_g")[0], "w_router": f("w_router")[0], "w_gate_e": f("w_gate_e")[0][:edecl],
        "w_up_e": f("w_up_e")[0][:edecl], "w_down_e": f("w_down_e")[0][:edecl],
        "ident": np.eye(128, dtype=np.float32),
        "offs": (2048.0 * ((np.arange(128) % 64) // 16) + 1024.0 * (np.arange(128) // 64)).astype(np.float32).reshape(128, 1),
        "flip": np.ascontiguousarray(np.eye(128, dtype=np.float32)[::-1]),
    }
    maps = []
    for c in range(ncores):
        m = dict(common)
        m["x"] = np.ascontiguousarray(x[c * nseq:(c + 1) * nseq].reshape(nseq * S, D))
        maps.append(m)
    return maps


_NC_CACHE = {}


def kernel(**inputs):
    nseq = 4
    if "full" not in _NC_CACHE:
        _NC_CACHE["full"] = build(NSEQ=nseq, upto="C", dbg=False)
    nc = _NC_CACHE["full"]
    in_maps = make_in_maps(inputs, NCORES, nseq)
    res = run_bass_kernel_spmd(nc, in_maps, core_ids=list(range(NCORES)))
    out = np.concatenate([np.asarray(r["out"]).reshape(nseq, S, D) for r in res.results], axis=0)
    return out.astype(np.float32)
```
